# Optimizing a Trainium2 kernel written in Bass

```python
import math
import jax, jax.numpy as jnp
from jax import lax
import numpy as np

D_MODEL = 1024
BATCH = 8
SEQ = 2048
DEPTH = 2

HEAD_DIM = 64
A_HEADS = 8
A_KV_HEADS = 2
WINDOW = 128
A_BLOCK = 128
B_HEADS = 4
B_BLOCK = 128
C_HEADS = 8
GRID_W = 64
NA_ROWS = 8
NA_COLS = 16
NA_COL_BLOCK = 16
NA_KEY_COLS = NA_COL_BLOCK + NA_COLS
MIX_W = 512
N_BRANCH = 3
N_EXPERTS = 32
TOP_K = 4
D_EXPERT = 1024
MOE_BLOCK = 128
SWIGLU_LIMIT = 7.0
SWIGLU_ALPHA = 1.702
LN_EPS = 1e-5
NEG_INF = -1e30
DEEPNORM_ALPHA = (2 * DEPTH) ** 0.25
DEEPNORM_BETA = (8 * DEPTH) ** -0.25

A_Q_W = A_HEADS * HEAD_DIM
A_KV_W = A_KV_HEADS * HEAD_DIM
B_QK_W = B_HEADS * 2 * HEAD_DIM
B_V_W = B_HEADS * 2 * HEAD_DIM
C_W = C_HEADS * HEAD_DIM
GATE_W = N_BRANCH * D_MODEL
IN_SPLITS = (A_Q_W, A_KV_W, A_KV_W, B_QK_W, B_QK_W, B_V_W, C_W, C_W, C_W, GATE_W)
IN_W = sum(IN_SPLITS)

kernel_name = "hybrid_gated_swa_diff_na_moe_encoder"


def layer_norm(x, g, b):
    xf = x.astype(jnp.float32)
    mu = jnp.mean(xf, axis=-1, keepdims=True)
    var = jnp.mean(jnp.square(xf - mu), axis=-1, keepdims=True)
    y = (xf - mu) * lax.rsqrt(var + LN_EPS) * g.astype(jnp.float32) + b.astype(jnp.float32)
    return y.astype(x.dtype)


def alibi_slopes():
    n = A_HEADS + B_HEADS
    return jnp.asarray(2.0 ** (-8.0 * np.arange(1, n + 1) / n), dtype=jnp.float32)


def windowed_gqa_sink(q, k, v, sink, slopes):
    bsz, seq = q.shape[0], q.shape[1]
    nb = seq // A_BLOCK
    grp = A_HEADS // A_KV_HEADS

    def band(t):
        tp = jnp.pad(t, ((0, 0), (A_BLOCK, A_BLOCK), (0, 0), (0, 0)))
        tp = tp.reshape(bsz, nb + 2, A_BLOCK, A_KV_HEADS, HEAD_DIM)
        return jnp.concatenate([tp[:, :-2], tp[:, 1:-1], tp[:, 2:]], axis=2)

    kb, vb = band(k), band(v)
    qb = q.reshape(bsz, nb, A_BLOCK, A_KV_HEADS, grp, HEAD_DIM)
    s = jnp.einsum('bnqgrd,bnkgd->bngrqk', qb, kb).astype(jnp.float32) * (HEAD_DIM ** -0.5)
    qi = np.arange(A_BLOCK)[:, None]
    ki = np.arange(3 * A_BLOCK)[None, :]
    rel = ki - A_BLOCK - qi
    kpos = (np.arange(nb)[:, None, None] - 1) * A_BLOCK + ki[None]
    valid = (np.abs(rel) <= WINDOW)[None] & (kpos >= 0) & (kpos < seq)
    dist = jnp.asarray(np.abs(rel), dtype=jnp.float32)
    bias = -slopes.reshape(A_KV_HEADS, grp, 1, 1) * dist
    s = jnp.where(valid[None, :, None, None], s + bias, NEG_INF)
    sink_logit = jnp.broadcast_to(sink.astype(jnp.float32).reshape(1, 1, A_KV_HEADS, grp, 1, 1),
                                  s.shape[:-1] + (1,))
    p = jax.nn.softmax(jnp.concatenate([s, sink_logit], axis=-1), axis=-1)[..., :-1]
    o = jnp.einsum('bngrqk,bnkgd->bnqgrd', p.astype(v.dtype), vb)
    return o.reshape(bsz, seq, A_Q_W)


def diff_attention(q, k, v, lam, lam_init, gain, slopes):
    bsz, seq = q.shape[0], q.shape[1]
    nb = seq // B_BLOCK
    kpos = jnp.arange(seq, dtype=jnp.float32)
    qblocks = q.reshape(bsz, nb, B_BLOCK, B_HEADS, 2, HEAD_DIM).swapaxes(0, 1)

    def block(args):
        qb, i = args
        qpos = i.astype(jnp.float32) * B_BLOCK + jnp.arange(B_BLOCK, dtype=jnp.float32)
        s = jnp.einsum('bqhmd,bkhmd->bhmqk', qb, k).astype(jnp.float32) * (HEAD_DIM ** -0.5)
        s = s - slopes[:, None, None, None] * jnp.abs(qpos[:, None] - kpos[None, :])
        p = jax.nn.softmax(s, axis=-1)
        a = p[:, :, 0] - lam * p[:, :, 1]
        return jnp.einsum('bhqk,bkhe->bqhe', a.astype(v.dtype), v)

    o = lax.map(block, (qblocks, jnp.arange(nb)))
    o = o.swapaxes(0, 1).reshape(bsz, seq, B_HEADS, 2 * HEAD_DIM).astype(jnp.float32)
    o = o * lax.rsqrt(jnp.mean(jnp.square(o), axis=-1, keepdims=True) + LN_EPS)
    o = o * gain.astype(jnp.float32) * (1.0 - lam_init)
    return o.astype(v.dtype).reshape(bsz, seq, B_V_W)


def neighbourhood_attention(q, k, v, rpb):
    bsz, seq = q.shape[0], q.shape[1]
    rows = seq // GRID_W
    kr = min(NA_ROWS, rows)
    ncb = GRID_W // NA_COL_BLOCK
    r = np.arange(rows)
    rs = np.clip(r - kr // 2, 0, rows - kr)
    rows_idx = rs[:, None] + np.arange(kr)[None, :]
    cbs = np.clip(np.arange(ncb) * NA_COL_BLOCK - NA_COLS // 2, 0, GRID_W - NA_KEY_COLS)
    cols_idx = cbs[:, None] + np.arange(NA_KEY_COLS)[None, :]
    qc = np.arange(ncb)[:, None] * NA_COL_BLOCK + np.arange(NA_COL_BLOCK)[None, :]
    cs = np.clip(qc - NA_COLS // 2, 0, GRID_W - NA_COLS)
    kc = cols_idx[:, None, :]
    valid = (kc >= cs[:, :, None]) & (kc < cs[:, :, None] + NA_COLS)
    dri = rows_idx - r[:, None] + NA_ROWS - 1
    dci = np.clip(kc - qc[:, :, None] + NA_COLS - 1, 0, 2 * NA_COLS - 2)

    qg = q.reshape(bsz, rows, ncb, NA_COL_BLOCK, C_HEADS, HEAD_DIM)
    kg = k.reshape(bsz, rows, GRID_W, C_HEADS, HEAD_DIM)
    vg = v.reshape(bsz, rows, GRID_W, C_HEADS, HEAD_DIM)
    ridx = rows_idx[:, None, :, None]
    cidx = cols_idx[None, :, None, :]
    kn = kg[:, ridx, cidx]
    vn = vg[:, ridx, cidx]
    s = jnp.einsum('brjqhd,brjuvhd->brjhquv', qg, kn).astype(jnp.float32) * (HEAD_DIM ** -0.5)
    bias = rpb.astype(jnp.float32)[:, dri[:, :, None, None, None], dci[None, None]]
    bias = bias.transpose(1, 3, 0, 4, 2, 5)
    s = jnp.where(valid[None, None, :, None, :, None, :], s + bias[None], NEG_INF)
    shp = s.shape
    p = jax.nn.softmax(s.reshape(shp[:-2] + (kr * NA_KEY_COLS,)), axis=-1).reshape(shp)
    o = jnp.einsum('brjhquv,brjuvhd->brjqhd', p.astype(v.dtype), vn)
    return o.reshape(bsz, seq, C_W)


def hybrid_mixer(h, layer, w_in, a_sink, lambda_q1, lambda_k1, lambda_q2, lambda_k2,
                 diff_norm_g, na_rpb, w_branch, w_out):
    bsz, seq, _ = h.shape
    proj = h @ w_in
    offs = np.cumsum(IN_SPLITS)[:-1].tolist()
    aq, ak, av, bq, bk, bv, cq, ck, cv, g = jnp.split(proj, offs, axis=-1)
    slopes = alibi_slopes()

    oa = windowed_gqa_sink(aq.reshape(bsz, seq, A_HEADS, HEAD_DIM),
                           ak.reshape(bsz, seq, A_KV_HEADS, HEAD_DIM),
                           av.reshape(bsz, seq, A_KV_HEADS, HEAD_DIM),
                           a_sink, slopes[:A_HEADS])

    lam_init = 0.8 - 0.6 * math.exp(-0.3 * layer)
    f32 = jnp.float32
    lam = (jnp.exp(jnp.sum(lambda_q1.astype(f32) * lambda_k1.astype(f32)))
           - jnp.exp(jnp.sum(lambda_q2.astype(f32) * lambda_k2.astype(f32))) + lam_init)
    ob = diff_attention(bq.reshape(bsz, seq, B_HEADS, 2, HEAD_DIM),
                        bk.reshape(bsz, seq, B_HEADS, 2, HEAD_DIM),
                        bv.reshape(bsz, seq, B_HEADS, 2 * HEAD_DIM),
                        lam, lam_init, diff_norm_g, slopes[A_HEADS:])

    oc = neighbourhood_attention(cq.reshape(bsz, seq, C_HEADS, HEAD_DIM),
                                 ck.reshape(bsz, seq, C_HEADS, HEAD_DIM),
                                 cv.reshape(bsz, seq, C_HEADS, HEAD_DIM), na_rpb)

    branches = jnp.stack([oa, ob, oc], axis=2)
    br = jnp.einsum('bsmc,mcd->bsmd', branches, w_branch)
    gates = jax.nn.sigmoid(g.reshape(bsz, seq, N_BRANCH, D_MODEL))
    merged = jnp.sum(gates * br, axis=2)
    return merged @ w_out


def moe_ffn(x, w_router, b_router, w_up, b_up, w_down, b_down):
    bsz, seq, d = x.shape
    n = bsz * seq
    xf = x.reshape(n, d)
    logits = (xf @ w_router + b_router).astype(jnp.float32)
    top_vals, top_idx = lax.top_k(logits, TOP_K)
    gate_w = jax.nn.softmax(top_vals, axis=-1)
    n_assign = n * TOP_K
    flat_e = top_idx.reshape(-1).astype(jnp.int32)
    flat_tok = (jnp.arange(n_assign, dtype=jnp.int32) // TOP_K)
    flat_w = gate_w.reshape(-1)
    order = jnp.argsort(flat_e)
    sorted_e = flat_e[order]
    counts = jnp.bincount(flat_e, length=N_EXPERTS)
    padded = (counts + MOE_BLOCK - 1) // MOE_BLOCK * MOE_BLOCK
    start = jnp.cumsum(counts) - counts
    pend = jnp.cumsum(padded)
    pstart = pend - padded
    dest = pstart[sorted_e] + (jnp.arange(n_assign, dtype=jnp.int32) - start[sorted_e])
    n_pad = n_assign + N_EXPERTS * MOE_BLOCK
    n_blk = n_pad // MOE_BLOCK
    tok_pad = jnp.full((n_pad,), n, dtype=jnp.int32).at[dest].set(flat_tok[order])
    w_pad = jnp.zeros((n_pad,), jnp.float32).at[dest].set(flat_w[order])
    blk_e = jnp.minimum(jnp.searchsorted(pend, jnp.arange(n_blk) * MOE_BLOCK, side='right'),
                        N_EXPERTS - 1).astype(jnp.int32)
    x_pad = jnp.concatenate([xf, jnp.zeros((1, d), xf.dtype)], axis=0)
    xs = x_pad[tok_pad].reshape(n_blk, MOE_BLOCK, d)

    def expert_block(args):
        xb, e = args
        hh = xb @ w_up[e] + b_up[e]
        hg, hl = hh[:, :D_EXPERT], hh[:, D_EXPERT:]
        hg = jnp.minimum(hg, SWIGLU_LIMIT)
        hl = jnp.clip(hl, -SWIGLU_LIMIT, SWIGLU_LIMIT)
        act = hg * jax.nn.sigmoid(SWIGLU_ALPHA * hg) * (hl + 1.0)
        return act @ w_down[e] + b_down[e]

    ys = lax.map(expert_block, (xs, blk_e)).reshape(n_pad, d)
    out = jnp.zeros((n + 1, d), ys.dtype).at[tok_pad].add(ys * w_pad[:, None].astype(ys.dtype))
    return out[:n].reshape(bsz, seq, d)


def setup_inputs(seed: int = 0) -> dict:
    key = jax.random.key(seed)
    ks = jax.random.split(key, 20)
    L, D, E, F = DEPTH, D_MODEL, N_EXPERTS, D_EXPERT
    nrm = jax.random.normal
    val_scale = [1.0, 1.0, DEEPNORM_BETA, 1.0, 1.0, DEEPNORM_BETA, 1.0, 1.0, DEEPNORM_BETA, 1.0]
    col_scale = jnp.asarray(np.concatenate([np.full(w, s, np.float32)
                                            for w, s in zip(IN_SPLITS, val_scale)]))
    return {
        "x": nrm(ks[0], (BATCH, SEQ, D), jnp.float32),
        "w_in": nrm(ks[1], (L, D, IN_W), jnp.float32) * (D ** -0.5) * col_scale,
        "a_sink": nrm(ks[2], (L, A_HEADS), jnp.float32) * 0.5,
        "lambda_q1": nrm(ks[3], (L, HEAD_DIM), jnp.float32) * 0.1,
        "lambda_k1": nrm(ks[4], (L, HEAD_DIM), jnp.float32) * 0.1,
        "lambda_q2": nrm(ks[5], (L, HEAD_DIM), jnp.float32) * 0.1,
        "lambda_k2": nrm(ks[6], (L, HEAD_DIM), jnp.float32) * 0.1,
        "diff_norm_g": 1.0 + 0.02 * nrm(ks[7], (L, 2 * HEAD_DIM), jnp.float32),
        "na_rpb": 0.05 * nrm(ks[8], (L, C_HEADS, 2 * NA_ROWS - 1, 2 * NA_COLS - 1), jnp.float32),
        "w_branch": nrm(ks[9], (L, N_BRANCH, MIX_W, D), jnp.float32) * (MIX_W ** -0.5),
        "w_out": nrm(ks[10], (L, D, D), jnp.float32) * (D ** -0.5) * DEEPNORM_BETA,
        "ln1_g": 1.0 + 0.02 * nrm(ks[11], (L, D), jnp.float32),
        "ln1_b": 0.02 * nrm(ks[12], (L, D), jnp.float32),
        "w_router": nrm(ks[13], (L, D, E), jnp.float32) * (D ** -0.5),
        "b_router": 0.01 * nrm(ks[14], (L, E), jnp.float32),
        "w_up": nrm(ks[15], (L, E, D, 2 * F), jnp.float32) * (D ** -0.5),
        "b_up": 0.01 * nrm(ks[16], (L, E, 2 * F), jnp.float32),
        "w_down": nrm(ks[17], (L, E, F, D), jnp.float32) * (F ** -0.5) * DEEPNORM_BETA,
        "b_down": 0.01 * nrm(ks[18], (L, E, D), jnp.float32),
        "ln2_g": 1.0 + 0.02 * nrm(ks[19], (L, D), jnp.float32),
        "ln2_b": 0.02 * nrm(jax.random.fold_in(ks[19], 1), (L, D), jnp.float32),
    }


def reference(x, w_in, a_sink, lambda_q1, lambda_k1, lambda_q2, lambda_k2, diff_norm_g, na_rpb,
              w_branch, w_out, ln1_g, ln1_b, w_router, b_router, w_up, b_up, w_down, b_down,
              ln2_g, ln2_b):
    for l in range(DEPTH):
        mix = hybrid_mixer(x, l, w_in[l], a_sink[l], lambda_q1[l], lambda_k1[l], lambda_q2[l],
                           lambda_k2[l], diff_norm_g[l], na_rpb[l], w_branch[l], w_out[l])
        x = layer_norm(DEEPNORM_ALPHA * x + mix, ln1_g[l], ln1_b[l])
        ffn = moe_ffn(x, w_router[l], b_router[l], w_up[l], b_up[l], w_down[l], b_down[l])
        x = layer_norm(DEEPNORM_ALPHA * x + ffn, ln2_g[l], ln2_b[l])
    return x
```

```python
import math
from contextlib import ExitStack
import numpy as np
import concourse.bass as bass
import concourse.mybir as mybir
from concourse.bass_utils import run_bass_kernel_spmd

F32 = mybir.dt.float32
BF16 = mybir.dt.bfloat16
ALU = mybir.AluOpType
AF = mybir.ActivationFunctionType
AX = mybir.AxisListType

T = 2048
D = 1024
NL = 2
NE = 32
ALPHA = (2 * NL) ** 0.25
LN_EPS = 1e-5
IN_W = 6912
NS = 4
NEGBIG = -30000.0
CS_SILU = 1.702 * 7.0 / (1.0 + math.exp(-1.702 * 7.0))
OFF_AQ, OFF_AK, OFF_AV = 0, 512, 640
OFF_BQ, OFF_BK, OFF_BV = 768, 1280, 1792
OFF_CQ, OFF_CK, OFF_CV = 2304, 2816, 3328
OFF_G = 3840
NCV = 33 + 512
NBR = 8 + 256 + 32 + 32
CAP = 384
NROW = NE * CAP + T
NSC = CAP // 128
I32 = mybir.dt.int32
WA, WB, WC = 1152, 3968, 22 * 64


class Sem:
    def __init__(self, h, name, total=False):
        self.h = h
        self.name = name
        self.n = 0
        self.total = total


class Buf:
    __slots__ = ("name", "w", "r")

    def __init__(self, name=""):
        self.name = name
        self.w = None
        self.r = {}


class Eng:
    def __init__(self, name, eng, sem, self_sync):
        self.name = name
        self.eng = eng
        self.sem = sem
        self.seen = {}
        self.self_sync = self_sync

    def wait(self, s, v):
        if s.total:
            v = max(v, s.n)
        if self.seen.get(s.name, 0) >= v:
            return
        self.eng.wait_ge(s.h, v)
        self.seen[s.name] = v


def _deps(E, reads, writes):
    deps = {}

    def need(tag):
        s, v = tag
        if s is E.sem and not E.self_sync:
            return
        if s.name not in deps or deps[s.name][1] < v:
            deps[s.name] = (s, v)

    for b in reads:
        if b.w is not None:
            need(b.w)
    for b in writes:
        if b.w is not None:
            need(b.w)
        for tag in b.r.values():
            need(tag)
    for s, v in deps.values():
        E.wait(s, v)


def op(E, reads, writes, fn):
    _deps(E, reads, writes)
    ins = fn()
    E.sem.n += 1
    ins.then_inc(E.sem.h, 1)
    tag = (E.sem, E.sem.n)
    for b in reads:
        b.r[E.sem.name] = tag
    for b in writes:
        b.w = tag
        b.r = {}


def dma(E, S, reads, writes, fn):
    _deps(E, reads, writes)
    ins = fn()
    S.n += 16
    ins.then_inc(S.h, 16)
    tag = (S, S.n)
    for b in reads:
        b.r[S.name] = tag
    for b in writes:
        b.w = tag
        b.r = {}


class Rot:
    def __init__(self, items):
        self.items = items
        self.i = 0

    def next(self):
        it = self.items[self.i % len(self.items)]
        self.i += 1
        return it


def build(stop_after=None, sparse=True):
    nc = bass.Bass("TRN2", target_bir_lowering=False)

    def din(name, shape):
        return nc.dram_tensor(name, list(shape), F32, kind="ExternalInput").ap()

    xT_d = din("xT", [D, T])
    w_in_d = din("w_in", [NL, D, IN_W])
    w_br_d = din("w_branch", [NL, 3, 512, D])
    w_out_d = din("w_out", [NL, D, D])
    w_rt_d = din("w_router", [NL, D, NE])
    NEW = 1 if (stop_after is not None and not stop_after.startswith("moe")) else NE
    w_up_d = din("w_up", [NL, NEW, D, 2 * D])
    w_dn_d = din("w_down", [NL, NEW, D, D])
    b_dn_d = din("b_down", [NL, NE, D])
    cvec_d = din("cvec", [NL, 128, NCV])
    brow_d = din("brow", [NL, 1, NBR])
    stripA_d = din("stripA", [128, WA])
    stripB_d = din("stripB", [128, WB])
    stripC_d = din("stripC", [NL, 8, 128, WC])
    nsl_d = din("nsl", [128, 13 * 128])
    ident_d = din("ident", [128, 128])
    tris_d = din("tris", [128, 128])
    rowoh_d = din("rowoh", [32, T])
    mrexp_d = din("mrexp", [32, T])
    out_d = nc.dram_tensor("outT", [D, T], F32, kind="ExternalOutput").ap()
    xr_d = [[nc.dram_tensor(f"xr{l}{k}", [D, T], F32, kind="Internal").ap() for k in range(2)] for l in range(NL)]
    gT_d = [nc.dram_tensor(f"gT{l}", [NE, T], F32, kind="Internal").ap() for l in range(NL)]
    flag_d = nc.dram_tensor("flag", [128, NL], F32, kind="ExternalOutput").ap()
    if sparse:
        xs_d = nc.dram_tensor("xs_scr", [NROW, D], BF16, kind="Internal").ap()
        ys_d = nc.dram_tensor("ys_scr", [NROW, D], F32, kind="Internal").ap()
    dbg_d = None
    if stop_after is not None:
        dbg_d = nc.dram_tensor("dbg", [128, 12 * T], BF16, kind="ExternalOutput").ap()
        dbg2_d = nc.dram_tensor("dbg2", [128, 8 * T], F32, kind="ExternalOutput").ap()

    with ExitStack() as es:
        EC = es.enter_context

        def sb(name, shape, dt):
            return EC(nc.sbuf_tensor("sb_" + name, list(shape), dt))

        def newsem(name, total=False):
            return Sem(EC(nc.semaphore(name)), name, total)

        PE = Eng("pe", nc.tensor, newsem("s_pe"), False)
        ACT = Eng("act", nc.scalar, newsem("s_act"), True)
        DVE = Eng("dve", nc.vector, newsem("s_dve"), True)
        POOL = Eng("pool", nc.gpsimd, newsem("s_pool"), True)
        SP = Eng("sp", nc.sync, newsem("s_sp"), True)
        ENGS = [PE, ACT, DVE, POOL, SP]
        semC = newsem("d_const", total=True)
        semCP = newsem("d_const_sw", total=True)
        semS = newsem("d_store", total=True)
        semG = newsem("d_gt", total=True)
        semX = newsem("d_xres")
        semGB = [newsem(f"d_gb{i}") for i in range(2)]
        semSC = [newsem(f"d_sc{i}") for i in range(2)]
        NSMAX = NS + 7
        semR = [newsem(f"d_ring{i}") for i in range(NSMAX)]
        semM = newsem("d_misc", total=True)
        semMP = newsem("d_misc_sw", total=True)
        semSCAT = newsem("d_scat", total=True)
        semYS = newsem("d_ys", total=True)
        semXL = [newsem(f"d_xl{i}") for i in range(2)]
        semYG = [newsem(f"d_yg{i}") for i in range(2)]
        DSEMS = [semC, semCP, semS, semG, semX, semM, semMP, semSCAT, semYS] + semGB + semSC + semR + semXL + semYG

        def barrier():
            for E in ENGS:
                for O in ENGS:
                    if O is not E and O.sem.n > 0:
                        E.wait(O.sem, O.sem.n)
                for S in DSEMS:
                    if S.n > 0:
                        E.wait(S, S.n)

        xb = sb("xb", [128, 8, T], BF16)
        xbB = [Buf(f"xb{t}") for t in range(4)]
        ring = sb("ring", [128, NS, 4096], BF16)
        ringB = [Buf(f"ring{i}") for i in range(NS + 7)]
        slots = [ring[:, i, :] for i in range(NS)]
        nsl = sb("nsl", [128, 13, 128], BF16)
        ident = sb("ident", [128, 128], F32)
        ones_bf = sb("ones_bf", [128, 128], BF16)
        ones_f = sb("ones_f", [128, 128], F32)
        cvec = sb("cvec", [128, NL, NCV], F32)
        brow = sb("brow", [128, NL, NBR], F32)
        smalls = sb("smalls", [128, 64], F32)
        bgl = sb("bgl", [128, NE, 16], F32)
        wr = sb("wr", [128, 8, NE], F32)
        tris = sb("tris", [128, 128], F32)
        cum = sb("cum", [128, NE], F32)
        ovmax = sb("ovmax", [128, NE], F32)
        D4 = sb("D4", [128, 16, 4], I32)
        G4 = sb("G4", [128, 16, 4], F32)
        flagt = sb("flagt", [128, NL], F32)
        cumB = Buf(); ovB = Buf(); flagB = Buf(); xsB = Buf(); ysB = Buf()
        d4Bs = [Buf() for _ in range(16)]
        g4Bs = [Buf() for _ in range(16)]
        constB = Buf("const")
        smallB = Buf("smalls")
        bglB = Buf("bgl")
        wrB = Buf("wr")
        ARENA_W = 31744
        arena = sb("arena", [128, ARENA_W], F32)

        def carve_f32(off, n):
            return arena[:, off:off + n]

        def carve_bf(off, n_bf):
            return arena[:, off:off + n_bf // 2].bitcast(BF16)

        psum = [EC(nc.psum_tensor(f"ps{i}", [128, 512], F32)) for i in range(8)]
        psB_all = [Buf(f"ps{i}") for i in range(8)]
        PS8 = Rot([(psum[i], psB_all[i]) for i in range(8)])
        PS4 = Rot([(psum[i], psB_all[i]) for i in range(4, 8)])
        PSACC = Rot([(psum[i], psB_all[i]) for i in range(4)])
        PSH = [PS8]

        class _PS:
            @staticmethod
            def next():
                return PSH[0].next()
        PS = _PS

        def cload(E, out_ap, in_ap):
            dma(E, (semCP if E is POOL else semC), [], [constB], lambda: E.eng.dma_start(out=out_ap, in_=in_ap))

        cload(POOL, nsl[:].rearrange("p a b -> p (a b)"), nsl_d[:, :])
        cload(SP, ident[:], ident_d[:, :])
        cload(SP, tris[:], tris_d[:, :])
        op(DVE, [], [flagB], lambda: nc.vector.memset(flagt[:], 0.0))
        for l in range(NL):
            cload(SP, cvec[:, l, :], cvec_d[l])
            cload(SP, brow[:, l, :], brow_d[l].partition_broadcast(128))
        op(DVE, [], [constB], lambda: nc.vector.memset(ones_f[:], 1.0))
        op(DVE, [], [constB], lambda: nc.vector.memset(ones_bf[:], 1.0))
        op(DVE, [], [smallB], lambda: nc.vector.memset(smalls[:, 0:1], LN_EPS))
        EPS = smalls[:, 0:1]
        for E_ in ENGS:
            E_.wait(semC, semC.n)
            E_.wait(semCP, semCP.n)

        ring_i = [0]

        def wload(pieces):
            s = ring_i[0] % len(slots)
            ring_i[0] += 1
            for (off, rows, ncols, src) in pieces:
                kc = rows // 128
                dst = slots[s][:, off:off + kc * ncols].rearrange("p (k n) -> p k n", n=ncols)
                dma(POOL, semR[s], [], [ringB[s]],
                    lambda dst=dst, src=src: nc.gpsimd.dma_start(out=dst, in_=src.rearrange("(k p) n -> p k n", p=128)))
            return s

        def rview(s, off, kc, ncols):
            return slots[s][:, off:off + kc * ncols].rearrange("p (k n) -> p k n", n=ncols)

        def mm_group(out_ap, psB, pairs, reads):
            def fn():
                ins = None
                n = len(pairs)
                for i, (l, r) in enumerate(pairs):
                    ins = nc.tensor.matmul(out_ap, lhsT=l, rhs=r, start=(i == 0), stop=(i == n - 1))
                return ins
            op(PE, reads, [psB], fn)

        def layer_norm_tile(l, which, tt, ytile, yB, xres_src_B, store_dst, st):
            gcol = 0 if which == 0 else 16
            tmp = st["lntmp"]
            s1, s1B = PS.next()
            s2, s2B = PS.next()
            mm_group(s1[:], s1B, [(ones_f[:], ytile[:, c, :]) for c in range(8)], yB + [constB])
            sqs = []
            for c in range(8):
                sq, sqB = tmp["sq"].next()
                op(ACT, [yB[c]], [sqB], lambda c=c, sq=sq: nc.scalar.activation(out=sq, in_=ytile[:, c, :], func=AF.Square))
                op(PE, [sqB, constB], [s2B],
                   lambda c=c, sq=sq: nc.tensor.matmul(s2[:], lhsT=ones_f[:], rhs=sq, start=(c == 0), stop=(c == 7)))
            mean, meanB = tmp["mean"]
            rstd, rstdB = tmp["rstd"]
            var, varB = tmp["var"]
            op(ACT, [s1B], [meanB], lambda: nc.scalar.activation(out=mean, in_=s1[:], func=AF.Identity, scale=1.0 / D))
            op(DVE, [meanB], [varB], lambda: nc.vector.tensor_tensor(out=var, in0=mean, in1=mean, op=ALU.mult))
            op(DVE, [s2B, varB], [varB],
               lambda: nc.vector.scalar_tensor_tensor(out=var, in0=s2[:], scalar=1.0 / D, in1=var, op0=ALU.mult, op1=ALU.subtract))
            op(ACT, [varB, smallB], [varB], lambda: nc.scalar.activation(out=var, in_=var, func=AF.Sqrt, bias=EPS, scale=1.0))
            op(DVE, [varB], [rstdB], lambda: nc.vector.reciprocal(out=rstd, in_=var))
            for c in range(8):
                yc = ytile[:, c, :]
                op(DVE, [meanB], [yB[c]], lambda yc=yc: nc.vector.tensor_tensor(out=yc, in0=yc, in1=mean, op=ALU.subtract))
            for c in range(8):
                yc = ytile[:, c, :]
                op(DVE, [rstdB], [yB[c]], lambda yc=yc: nc.vector.tensor_tensor(out=yc, in0=yc, in1=rstd, op=ALU.mult))
            for c in range(8):
                yc = ytile[:, c, :]
                op(ACT, [constB], [yB[c]],
                   lambda yc=yc, c=c: nc.scalar.activation(out=yc, in_=yc, func=AF.Identity,
                                                           bias=cvec[:, l, gcol + 8 + c:gcol + 9 + c],
                                                           scale=cvec[:, l, gcol + c:gcol + c + 1]))
            for c in range(8):
                yc = ytile[:, c, :]
                op(ACT, [yB[c]], [xbB[tt]],
                   lambda yc=yc, c=c: nc.scalar.copy(out=xb[:, c, tt * 512:(tt + 1) * 512], in_=yc))
            dma(SP, semS, yB, [st["xrB"]],
                lambda: nc.sync.dma_start(out=store_dst.rearrange("(c p) n -> p c n", p=128)[:, :, tt * 512:(tt + 1) * 512],
                                          in_=ytile))

        if sparse:
            zsrc = ring[:, NS - 1, :]
            op(DVE, [], [ringB[NS - 1]], lambda: nc.vector.memset(zsrc, 0.0))
            for i in range(NE):
                dma(SP, semM, [ringB[NS - 1]], [xsB], lambda i=i: nc.sync.dma_start(
                    out=xs_d[i * CAP:(i + 1) * CAP, :].rearrange("(s p) k -> p s k", p=128),
                    in_=zsrc.rearrange("p (s k) -> p s k", k=1024)[:, 0:NSC, :]))

        for tt in range(4):
            dma(POOL, semMP, [], [xbB[tt]],
                lambda tt=tt: nc.gpsimd.dma_start(out=xb[:, :, tt * 512:(tt + 1) * 512],
                                                  in_=xT_d.rearrange("(c p) n -> p c n", p=128)[:, :, tt * 512:(tt + 1) * 512]))

        prev_xrB = None
        for l in range(NL):
            lam_init = 0.8 - 0.6 * math.exp(-0.3 * l)
            res_src = xT_d if l == 0 else xr_d[l - 1][1]
            o = 0
            OT = carve_bf(o, 12 * T).rearrange("p (a n) -> p a n", n=T); o += 12 * T // 2
            stripA = carve_bf(o, WA); o += WA // 2
            stripB = carve_bf(o, WB); o += WB // 2
            R1 = o
            QT = [carve_bf(o + i * (T // 2), T) for i in range(2)]; o += T
            KT = [carve_bf(o + i * (T // 2), T) for i in range(2)]; o += T
            Vall = carve_bf(o, 16 * 512).rearrange("p (a n) -> p a n", n=512); o += 16 * 512 // 2
            Et = [carve_bf(o + i * 256, 512) for i in range(4)]; o += 4 * 256
            sC = [carve_bf(o + i * (WC // 2), WC) for i in range(2)]; o += WC
            ftmp = [carve_f32(o + i * 512, 512) for i in range(8)]; o += 8 * 512
            assert o <= ARENA_W, o
            otB = [[[Buf() for _ in range(4)] for _ in range(4)] for _ in range(3)]
            qB = [[Buf() for _ in range(4)] for _ in range(2)]
            kB = [Buf() for _ in range(2)]
            vB = [Buf() for _ in range(16)]
            ER = Rot([(Et[i], Buf()) for i in range(4)])
            sCB = [Buf(), Buf()]
            FT = Rot([(ftmp[i], Buf()) for i in range(8)])
            stripsB = Buf()

            dma(POOL, semMP, [], [stripsB], lambda: nc.gpsimd.dma_start(out=stripA, in_=stripA_d[:, :]))
            dma(POOL, semMP, [], [stripsB], lambda: nc.gpsimd.dma_start(out=stripB, in_=stripB_d[:, :]))

            cb = brow[:, l, :]
            sc1, sc1B = FT.next()
            op(DVE, [constB], [sc1B], lambda: nc.vector.tensor_tensor(out=sc1[:, 0:64], in0=cb[:, 8:72], in1=cb[:, 72:136], op=ALU.mult))
            op(DVE, [constB, sc1B], [sc1B], lambda: nc.vector.tensor_tensor(out=sc1[:, 64:128], in0=cb[:, 136:200], in1=cb[:, 200:264], op=ALU.mult))
            op(DVE, [sc1B], [sc1B], lambda: nc.vector.reduce_sum(out=sc1[:, 128:129], in_=sc1[:, 0:64], axis=AX.X))
            op(DVE, [sc1B], [sc1B], lambda: nc.vector.reduce_sum(out=sc1[:, 129:130], in_=sc1[:, 64:128], axis=AX.X))
            op(ACT, [sc1B], [sc1B], lambda: nc.scalar.activation(out=sc1[:, 130:132], in_=sc1[:, 128:130], func=AF.Exp))
            op(DVE, [sc1B], [sc1B], lambda: nc.vector.tensor_tensor(out=sc1[:, 132:133], in0=sc1[:, 131:132], in1=sc1[:, 130:131], op=ALU.subtract))
            op(DVE, [sc1B, smallB], [smallB], lambda: nc.vector.tensor_scalar(out=smalls[:, 1:2], in0=sc1[:, 132:133], scalar1=-lam_init, scalar2=None, op0=ALU.add))
            op(DVE, [constB, smallB], [smallB], lambda: nc.vector.tensor_scalar(out=smalls[:, 2:3], in0=cvec[:, l, 32:33], scalar1=(1.0 - lam_init), scalar2=None, op0=ALU.mult))
            op(ACT, [constB, smallB], [smallB], lambda: nc.scalar.activation(out=smalls[:, 8:16], in_=cb[:, 0:8], func=AF.Exp))
            NEGLAM = smalls[:, 1:2]
            GAINB = smalls[:, 2:3]

            win = w_in_d[l]
            PSH[0] = PS4

            def project_V(s, voff, ncols):
                for blk in range(16):
                    ps, psB = PS.next()
                    mm_group(ps[:, 0:ncols], psB,
                             [(xb[:, kc, blk * 128:(blk + 1) * 128], rview(s, 0, 8, 512)[:, kc, voff:voff + ncols]) for kc in range(8)],
                             [xbB[blk // 4], ringB[s]])
                    op(DVE, [psB], [vB[blk]], lambda ps=ps, blk=blk: nc.vector.tensor_copy(out=Vall[:, blk, 0:ncols], in_=ps[:, 0:ncols]))

            def project_T(s, coff, m, dst, dstB_list, scale, whole_buf=None):
                for tt in range(4):
                    ps, psB = PS.next()
                    mm_group(ps[0:m, :], psB,
                             [(rview(s, 0, 8, 512)[:, kc, coff:coff + m], xb[:, kc, tt * 512:(tt + 1) * 512]) for kc in range(8)],
                             [xbB[tt], ringB[s]])
                    wB = whole_buf if whole_buf is not None else dstB_list[tt]
                    if scale == 1.0:
                        op(DVE, [psB], [wB], lambda ps=ps, tt=tt: nc.vector.tensor_copy(out=dst[0:m, tt * 512:(tt + 1) * 512], in_=ps[0:m, :]))
                    else:
                        op(DVE, [psB], [wB], lambda ps=ps, tt=tt: nc.vector.tensor_scalar(out=dst[0:m, tt * 512:(tt + 1) * 512], in0=ps[0:m, :],
                                                                                         scalar1=scale, scalar2=None, op0=ALU.mult))

            def project_pair(s, coff, dsts, scale):
                for tt in range(4):
                    ps, psB = PS.next()
                    mm_group(ps[:, :], psB,
                             [(rview(s, 0, 8, 512)[:, kc, coff:coff + 128], xb[:, kc, tt * 512:(tt + 1) * 512]) for kc in range(8)],
                             [xbB[tt], ringB[s]])
                    for i, (dst, blist, whole) in enumerate(dsts):
                        wB = whole if whole is not None else blist[tt]
                        src = ps[i * 64:(i + 1) * 64, :]
                        if scale == 1.0:
                            op(DVE, [psB], [wB], lambda dst=dst, src=src, tt=tt: nc.vector.tensor_copy(out=dst[0:64, tt * 512:(tt + 1) * 512], in_=src))
                        else:
                            op(DVE, [psB], [wB], lambda dst=dst, src=src, tt=tt: nc.vector.tensor_scalar(
                                out=dst[0:64, tt * 512:(tt + 1) * 512], in0=src, scalar1=scale, scalar2=None, op0=ALU.mult))

            pending_fin = []

            def flush_fin():
                while pending_fin:
                    pending_fin.pop(0)()

            def attn_tile(kbs, kparts, qbuf, qBt, kbuf, kBk, bias_l, strip_ap_fn, stripBuf, v_fn, dv, t, fin_cb):
                num, numB = PSACC.next()
                es, esB = FT.next()
                nk = len(kbs)
                pend = []
                lo, hi = kparts
                for i, kb in enumerate(kbs):
                    S, SB = PS.next()

                    def fs(S=S, kb=kb):
                        nc.tensor.matmul(S[:], lhsT=kbuf[lo:hi, kb * 128:(kb + 1) * 128], rhs=qbuf[lo:hi, t * 512:(t + 1) * 512],
                                         start=True, stop=False)
                        return nc.tensor.matmul(S[:], lhsT=bias_l, rhs=strip_ap_fn(kb), start=False, stop=True)
                    op(PE, [kBk, qBt, constB, stripBuf], [SB], fs)
                    if i == 0:
                        flush_fin()
                    E, EB = ER.next()
                    op(ACT, [SB], [EB], lambda S=S, E=E: nc.scalar.activation(out=E, in_=S[:], func=AF.Exp))
                    if i == 0:
                        op(DVE, [EB], [esB], lambda E=E: nc.vector.tensor_copy(out=es, in_=E))
                    else:
                        op(DVE, [EB, esB], [esB], lambda E=E: nc.vector.tensor_tensor(out=es, in0=E, in1=es, op=ALU.add))
                    pend.append((i, kb, E, EB))
                    if len(pend) == 2 or i == nk - 1:
                        while pend and (len(pend) == 2 or i == nk - 1):
                            (pi, pkb, pE, pEB) = pend.pop(0)
                            op(PE, [pEB, vB[pkb]], [numB],
                               lambda pi=pi, pkb=pkb, pE=pE: nc.tensor.matmul(num[0:dv, :], lhsT=v_fn(pkb), rhs=pE, start=(pi == 0), stop=(pi == nk - 1)))

                def fin():
                    den, denB = PS.next()
                    op(PE, [esB, constB], [denB], lambda: nc.tensor.matmul(den[0:dv, :], lhsT=ones_f[:, 0:dv], rhs=es, start=True, stop=True))
                    fin_cb(num, numB, den, denB)
                pending_fin.append(fin)

            sA1 = wload([(0, D, 512, win[:, OFF_AQ:OFF_AQ + 512])])
            sA2 = wload([(0, D, 512, win[:, OFF_AK:OFF_AK + 512])])
            project_V(sA2, 128, 128)
            project_pair(sA2, 0, [(KT[0], None, kB[0]), (KT[1], None, kB[1])], 1.0)
            for g in range(2):
                for r in range(4):
                    h = g * 4 + r
                    qb = h % 2
                    if qb == 0:
                        project_pair(sA1, h * 64, [(QT[0], qB[0], None), (QT[1], qB[1], None)], 0.125)
                    for t in range(4):
                        kbs = [kb for kb in range(4 * t - 1, 4 * t + 5) if 0 <= kb < 16]

                        def finA(num, numB, den, denB, h=h, t=t):
                            rd, rdB = FT.next()
                            op(DVE, [denB, smallB], [rdB], lambda: nc.vector.tensor_scalar(
                                out=rd[0:64, :], in0=den[0:64, :], scalar1=smalls[0:64, 8 + h:9 + h], scalar2=None, op0=ALU.add))
                            op(DVE, [rdB], [rdB], lambda: nc.vector.reciprocal(out=rd[0:64, :], in_=rd[0:64, :]))
                            pb = (h % 2) * 64
                            op(DVE, [numB, rdB], [otB[0][h // 2][t]], lambda: nc.vector.tensor_tensor(
                                out=OT[pb:pb + 64, h // 2, t * 512:(t + 1) * 512], in0=num[0:64, :], in1=rd[0:64, :], op=ALU.mult))
                        attn_tile(
                            kbs, (0, 64), QT[qb], qB[qb][t], KT[g % 2], kB[g % 2], nsl[:, h, :],
                            lambda kb, t=t: stripA[:, 512 - (128 * kb - 512 * t):512 - (128 * kb - 512 * t) + 512], stripsB,
                            lambda kb, g=g: Vall[:, kb, g * 64:(g + 1) * 64], 64, t, finA)
            flush_fin()

            sB1 = wload([(0, D, 512, win[:, OFF_BQ:OFF_BQ + 512])])
            sB2 = wload([(0, D, 512, win[:, OFF_BK:OFF_BK + 512])])
            sB3 = wload([(0, D, 512, win[:, OFF_BV:OFF_BV + 512])])
            project_V(sB3, 0, 512)
            om_store = {}
            for h in range(4):
                hb = h % 2
                project_T(sB2, h * 128, 128, KT[hb], None, 1.0, whole_buf=kB[hb])
                project_T(sB1, h * 128, 128, QT[hb], qB[hb], 0.125)
                for t in range(4):
                    for m in range(2):
                        def finB(num, numB, den, denB, h=h, t=t, m=m):
                            om, omB = FT.next()
                            op(DVE, [denB], [omB], lambda: nc.vector.reciprocal(out=om, in_=den[:]))
                            op(DVE, [numB, omB], [omB], lambda: nc.vector.tensor_tensor(out=om, in0=num[:], in1=om, op=ALU.mult))
                            om_store[(h, t, m)] = (om, omB)
                            if m == 0:
                                return
                            (o0, o0B) = om_store.pop((h, t, 0))
                            (o1, o1B) = om_store.pop((h, t, 1))
                            op(DVE, [o1B, smallB], [o0B], lambda: nc.vector.scalar_tensor_tensor(
                                out=o0, in0=o1, scalar=NEGLAM, in1=o0, op0=ALU.mult, op1=ALU.add))
                            op(ACT, [o0B], [o1B], lambda: nc.scalar.activation(out=o1, in_=o0, func=AF.Square))
                            ss, ssB = PS.next()
                            op(PE, [o1B, constB], [ssB], lambda: nc.tensor.matmul(ss[:], lhsT=ones_f[:], rhs=o1, start=True, stop=True))
                            op(ACT, [ssB, smallB], [o1B], lambda: nc.scalar.activation(out=o1, in_=ss[:], func=AF.Sqrt, bias=EPS, scale=1.0 / 128))
                            op(DVE, [o1B], [o1B], lambda: nc.vector.reciprocal(out=o1, in_=o1))
                            op(DVE, [o0B, o1B, smallB], [otB[1][h][t]], lambda: nc.vector.scalar_tensor_tensor(
                                out=OT[:, 4 + h, t * 512:(t + 1) * 512], in0=o0, scalar=GAINB, in1=o1, op0=ALU.mult, op1=ALU.mult))
                        attn_tile(
                            list(range(16)), (m * 64, m * 64 + 64), QT[hb], qB[hb][t], KT[hb], kB[hb], nsl[:, 8 + h, :],
                            lambda kb, t=t: stripB[:, 1920 - (128 * kb - 512 * t):1920 - (128 * kb - 512 * t) + 512], stripsB,
                            lambda kb, h=h: Vall[:, kb, h * 128:(h + 1) * 128], 128, t, finB)
            flush_fin()

            for i in range(2):
                dma(POOL, semMP, [], [kB[i]], lambda i=i: nc.gpsimd.dma_start(out=KT[i][64:96, :], in_=rowoh_d[:, :]))
                for tt in range(4):
                    dma(POOL, semMP, [], [qB[i][tt]], lambda i=i, tt=tt: nc.gpsimd.dma_start(
                        out=QT[i][64:96, tt * 512:(tt + 1) * 512], in_=mrexp_d[:, tt * 512:(tt + 1) * 512]))
            sC1 = wload([(0, D, 512, win[:, OFF_CQ:OFF_CQ + 512])])
            sC2 = wload([(0, D, 512, win[:, OFF_CK:OFF_CK + 512])])
            sC3 = wload([(0, D, 512, win[:, OFF_CV:OFF_CV + 512])])
            project_V(sC3, 0, 512)
            for h in range(8):
                hb = h % 2
                if hb == 0:
                    flush_fin()
                dma(POOL, semSC[hb], [], [sCB[hb]], lambda h=h, hb=hb: nc.gpsimd.dma_start(out=sC[hb], in_=stripC_d[l, h]))
                if hb == 0:
                    project_pair(sC2, h * 64, [(KT[0], None, kB[0]), (KT[1], None, kB[1])], 1.0)
                    project_pair(sC1, h * 64, [(QT[0], qB[0], None), (QT[1], qB[1], None)], 0.125)
                for t in range(4):
                    kbs = [kb for kb in range(4 * t - 2, 4 * t + 6) if 0 <= kb < 16]

                    def finC(num, numB, den, denB, h=h, t=t):
                        rd, rdB = FT.next()
                        op(DVE, [denB], [rdB], lambda: nc.vector.reciprocal(out=rd[0:64, :], in_=den[0:64, :]))
                        pb = (h % 2) * 64
                        op(DVE, [numB, rdB], [otB[2][h // 2][t]], lambda: nc.vector.tensor_tensor(
                            out=OT[pb:pb + 64, 8 + h // 2, t * 512:(t + 1) * 512], in0=num[0:64, :], in1=rd[0:64, :], op=ALU.mult))
                    attn_tile(
                        kbs, (0, 96), QT[hb], qB[hb][t], KT[hb], kB[hb], nsl[:, 12, :],
                        lambda kb, t=t, hb=hb: sC[hb][:, (10 - (2 * kb - 8 * t)) * 64:(10 - (2 * kb - 8 * t)) * 64 + 512], sCB[hb],
                        lambda kb, h=h: Vall[:, kb, h * 64:(h + 1) * 64], 64, t, finC)
            flush_fin()

            if stop_after == f"attn{l}":
                barrier()
                dma(SP, semS, [], [], lambda: nc.sync.dma_start(out=dbg_d[:, :], in_=OT.rearrange("p a n -> p (a n)")))
                SP.wait(semS, semS.n)
                return nc

            barrier()
            PSH[0] = PS8
            mg = carve_bf(R1, 8 * T).rearrange("p (a n) -> p a n", n=T)
            o = R1 + 8 * T // 2
            mtmp = [carve_f32(o + i * 512, 512) for i in range(8)]; o += 8 * 512
            assert o <= ARENA_W
            MT = Rot([(mtmp[i], Buf()) for i in range(8)])
            mgB = [[Buf() for _ in range(4)] for _ in range(8)]
            for c in range(8):
                sG = wload([(i * 1024, D, 128, win[:, OFF_G + i * 1024 + c * 128:OFF_G + i * 1024 + (c + 1) * 128]) for i in range(3)])
                sW = wload([(i * 512, 512, 128, w_br_d[l, i][:, c * 128:(c + 1) * 128]) for i in range(3)])
                for tt in range(4):
                    prods = []
                    for i in range(3):
                        pg, pgB = PS.next()
                        mm_group(pg[:], pgB, [(rview(sG, i * 1024, 8, 128)[:, kc, :], xb[:, kc, tt * 512:(tt + 1) * 512]) for kc in range(8)],
                                 [ringB[sG], xbB[tt]])
                        pbp, pbB = PS.next()
                        mm_group(pbp[:], pbB, [(rview(sW, i * 512, 4, 128)[:, kc, :], OT[:, i * 4 + kc, tt * 512:(tt + 1) * 512]) for kc in range(4)],
                                 [ringB[sW]] + [otB[i][kc][tt] for kc in range(4)])
                        sg, sgB = MT.next()
                        op(ACT, [pgB], [sgB], lambda pg=pg, sg=sg: nc.scalar.activation(out=sg, in_=pg[:], func=AF.Sigmoid))
                        op(DVE, [pbB, sgB], [sgB], lambda pbp=pbp, sg=sg: nc.vector.tensor_tensor(out=sg, in0=pbp[:], in1=sg, op=ALU.mult))
                        prods.append((sg, sgB))
                    (p0, p0B), (p1, p1B), (p2, p2B) = prods
                    op(DVE, [p0B, p1B], [p0B], lambda p0=p0, p1=p1: nc.vector.tensor_tensor(out=p0, in0=p0, in1=p1, op=ALU.add))
                    op(DVE, [p0B, p2B], [mgB[c][tt]], lambda p0=p0, p2=p2, c=c, tt=tt: nc.vector.tensor_tensor(
                        out=mg[:, c, tt * 512:(tt + 1) * 512], in0=p0, in1=p2, op=ALU.add))

            barrier()
            o = 0
            ytile = carve_f32(o, 8 * 512).rearrange("p (c n) -> p c n", n=512); o += 8 * 512
            xres = carve_f32(o, 8 * 512).rearrange("p (c n) -> p c n", n=512); o += 8 * 512
            lt = [carve_f32(o + i * 512, 512) for i in range(5)]; o += 5 * 512
            rts = [carve_f32(o + i * 512, 512) for i in range(4)]; o += 4 * 512
            rtBs = [Buf() for _ in range(4)]
            xtl = [carve_bf(o + i * 512, 1024) for i in range(4)]; o += 4 * 512
            assert o <= R1, o
            XTR = Rot([(xtl[i], Buf()) for i in range(4)])
            if sparse:
                op(DVE, [cumB], [cumB], lambda: nc.vector.memset(cum[:], 0.0))
                op(DVE, [ovB], [ovB], lambda: nc.vector.memset(ovmax[:], 0.0))
            yB = [Buf() for _ in range(8)]
            xresB = Buf()
            lntmp = {"sq": Rot([(lt[0], Buf()), (lt[1], Buf())]), "mean": (lt[2], Buf()), "rstd": (lt[3], Buf()), "var": (lt[4], Buf())}
            GT_OFF = 8 * T + 8 * T // 2
            assert GT_OFF >= R1 + 8 * T // 2
            GT = carve_f32(GT_OFF, T)
            GTB = Buf()
            sO = [wload([(0, D, 512, w_out_d[l][:, d * 512:(d + 1) * 512])]) for d in range(2)]
            dma(SP, semM, [], [wrB], lambda: nc.sync.dma_start(out=wr[:], in_=w_rt_d[l].rearrange("(c p) e -> p c e", p=128)))
            xr1B = Buf()
            st1 = {"lntmp": lntmp, "xrB": xr1B}
            for tt in range(4):
                dma(SP, semX, ([prev_xrB] if prev_xrB is not None else []), [xresB], lambda tt=tt: nc.sync.dma_start(
                    out=xres, in_=res_src.rearrange("(c p) n -> p c n", p=128)[:, :, tt * 512:(tt + 1) * 512]))
                for c in range(8):
                    ps, psB = PS.next()
                    mm_group(ps[:], psB, [(rview(sO[c // 4], 0, 8, 512)[:, kc, (c % 4) * 128:(c % 4 + 1) * 128], mg[:, kc, tt * 512:(tt + 1) * 512])
                                          for kc in range(8)], [ringB[sO[c // 4]]] + [mgB[kc][tt] for kc in range(8)])
                    op(DVE, [psB, xresB], [yB[c]], lambda ps=ps, c=c: nc.vector.scalar_tensor_tensor(
                        out=ytile[:, c, :], in0=xres[:, c, :], scalar=ALPHA, in1=ps[:], op0=ALU.mult, op1=ALU.add))
                layer_norm_tile(l, 0, tt, ytile, yB, None, xr_d[l][0], st1)
                def router_sub(sub, tt=tt):
                    rt = rts[sub]
                    rtB = rtBs[sub]
                    lg, lgB = PS.next()
                    mm_group(lg[:, 0:NE], lgB, [(ytile[:, c, sub * 128:(sub + 1) * 128], wr[:, c, :]) for c in range(8)], yB + [wrB])
                    LG = rt[:, 0:32]; T8 = rt[:, 32:40]; NM = rt[:, 40:41]; EX = rt[:, 64:96]; GX = rt[:, 96:128]; DN = rt[:, 41:42]
                    op(DVE, [lgB, constB], [rtB], lambda: nc.vector.tensor_tensor(out=LG, in0=lg[:, 0:NE], in1=brow[:, l, 264:296], op=ALU.add))
                    yield
                    op(DVE, [rtB], [rtB], lambda: nc.vector.max(out=T8, in_=LG))
                    yield
                    op(DVE, [rtB], [rtB], lambda: nc.vector.tensor_scalar(out=NM, in0=T8[:, 0:1], scalar1=-1.0, scalar2=None, op0=ALU.mult))
                    yield
                    op(ACT, [rtB], [rtB], lambda: nc.scalar.activation(out=EX, in_=LG, func=AF.Exp, bias=NM, scale=1.0))
                    col = tt * 512 + sub * 128
                    sidx = tt * 4 + sub
                    if sparse:
                        MK = rt[:, 128:160]; VAL = rt[:, 160:192]; V8 = rt[:, 192:200]; OH = rt[:, 200:232]; PM = rt[:, 232:264]; D4F = rt[:, 264:268]
                        op(DVE, [rtB], [rtB], lambda: nc.vector.tensor_scalar(out=MK, in0=LG, scalar1=T8[:, 3:4], scalar2=None, op0=ALU.is_ge))
                        pp, ppB = PS.next()

                        def fpos():
                            nc.tensor.matmul(pp[:, 0:NE], lhsT=ones_f[:], rhs=cum[:], start=True, stop=False)
                            return nc.tensor.matmul(pp[:, 0:NE], lhsT=tris[:], rhs=MK, start=False, stop=True)
                        op(PE, [rtB, cumB, constB], [ppB], fpos)
                        op(DVE, [rtB, cumB], [cumB], lambda: nc.vector.tensor_tensor(out=cum[:], in0=cum[:], in1=MK, op=ALU.add))
                    yield
                    op(DVE, [rtB], [rtB], lambda: nc.vector.scalar_tensor_tensor(out=GX, in0=LG, scalar=T8[:, 3:4], in1=EX, op0=ALU.is_ge, op1=ALU.mult))
                    yield
                    op(DVE, [rtB], [rtB], lambda: nc.vector.reduce_sum(out=DN, in_=GX, axis=AX.X))
                    yield
                    op(DVE, [rtB], [rtB], lambda: nc.vector.reciprocal(out=DN, in_=DN))
                    yield
                    op(DVE, [rtB], [rtB], lambda: nc.vector.tensor_scalar(out=GX, in0=GX, scalar1=DN, scalar2=None, op0=ALU.mult))
                    yield
                    tp, tpB = PS.next()
                    op(PE, [rtB, constB], [tpB], lambda: nc.tensor.transpose(tp[0:NE, 0:128], GX, ident[:]))
                    yield
                    op(ACT, [tpB], [GTB], lambda: nc.scalar.copy(out=GT[0:NE, col:col + 128], in_=tp[0:NE, 0:128]))
                    if sparse:
                        op(DVE, [ppB, rtB], [rtB], lambda: nc.vector.tensor_tensor(out=PM, in0=pp[:, 0:NE], in1=MK, op=ALU.mult))
                        yield
                        op(DVE, [rtB, ovB], [ovB], lambda: nc.vector.tensor_tensor(out=ovmax[:], in0=ovmax[:], in1=PM, op=ALU.max))
                        op(DVE, [ppB, constB, rtB], [rtB], lambda: nc.vector.tensor_tensor(out=VAL, in0=pp[:, 0:NE], in1=brow[:, l, 296:328], op=ALU.add))
                        yield
                        op(DVE, [rtB], [rtB], lambda: nc.vector.tensor_tensor(out=VAL, in0=VAL, in1=MK, op=ALU.mult))
                        yield
                        op(DVE, [rtB], [rtB], lambda: nc.vector.max(out=V8, in_=VAL))
                        yield
                        op(DVE, [rtB], [rtB], lambda: nc.vector.tensor_scalar(out=D4F, in0=V8[:, 0:4], scalar1=-1.0, scalar2=None, op0=ALU.add))
                        yield
                        op(DVE, [rtB], [d4Bs[sidx]], lambda: nc.vector.tensor_copy(out=D4[:, sidx, :], in_=D4F))
                        tq, tqB = PS.next()
                        tqb = tq[:].bitcast(BF16)

                        def ftr():
                            ins = None
                            for c in range(8):
                                ins = nc.tensor.transpose(tqb[:, c * 128:(c + 1) * 128], xb[:, c, col:col + 128], nsl[:, 12, :])
                            return ins
                        op(PE, [xbB[tt], constB], [tqB], ftr)
                        yield
                        xt, xtB = XTR.next()
                        op(ACT, [tqB], [xtB], lambda: nc.scalar.copy(out=xt, in_=tqb))
                        for k in range(4):
                            op(DVE, [rtB], [rtB], lambda k=k: nc.vector.tensor_scalar(out=OH, in0=VAL, scalar1=V8[:, k:k + 1], scalar2=None, op0=ALU.is_equal))
                            yield
                            op(DVE, [rtB], [rtB], lambda: nc.vector.tensor_tensor(out=OH, in0=OH, in1=GX, op=ALU.mult))
                            yield
                            op(DVE, [rtB], [g4Bs[sidx]], lambda k=k: nc.vector.reduce_sum(out=G4[:, sidx, k:k + 1], in_=OH, axis=AX.X))
                            yield
                        for k in range(4):
                            dma(POOL, semSCAT, [xtB, d4Bs[sidx]], [xsB], lambda k=k: nc.gpsimd.indirect_dma_start(
                                out=xs_d[:, :], out_offset=bass.IndirectOffsetOnAxis(ap=D4[:, sidx, k:k + 1], axis=0), in_=xt, in_offset=None))

                gens = [router_sub(sub) for sub in range(4)]
                while gens:
                    for g_ in list(gens):
                        try:
                            next(g_)
                        except StopIteration:
                            gens.remove(g_)
            gTB = Buf()
            dma(SP, semG, [GTB], [gTB], lambda: nc.sync.dma_start(out=gT_d[l][:, :], in_=GT[0:NE, :]))
            if sparse:
                op(DVE, g4Bs, g4Bs, lambda: nc.vector.tensor_scalar(out=G4[:], in0=G4[:], scalar1=1.0 / 1.702, scalar2=None, op0=ALU.mult))
                op(DVE, [ovB], [ovB], lambda: nc.vector.reduce_max(out=ovmax[:, 0:1], in_=ovmax[:], axis=AX.X))
                op(DVE, [ovB, flagB], [flagB], lambda: nc.vector.tensor_copy(out=flagt[:, l:l + 1], in_=ovmax[:, 0:1]))

            if stop_after == f"ln1{l}":
                barrier()
                dma(SP, semS, [], [], lambda: nc.sync.dma_start(out=dbg_d[:, 0:8 * T], in_=xb[:].rearrange("p a n -> p (a n)")))
                dma(SP, semS, [], [], lambda: nc.sync.dma_start(out=dbg2_d[0:NE, 0:T], in_=GT[0:NE, :]))
                SP.wait(semS, semS.n)
                return nc

            if sparse:
                barrier()
                acc = carve_f32(0, 8 * T).rearrange("p (c n) -> p c n", n=T)
                for i in range(7):
                    slots.append(carve_bf(i * 2048, 4096))
                xsT = [carve_bf(16384 + i * 2048, 8 * CAP).rearrange("p (c n) -> p c n", n=CAP) for i in range(2)]
                xsl = [carve_bf(20480 + i * 2048, NSC * 1024).rearrange("p (s k) -> p s k", k=1024) for i in range(2)]
                assert GT_OFF == 24576
                actT = carve_bf(14336, 8 * CAP).rearrange("p (c n) -> p c n", n=CAP)
                ysh = [carve_f32(26624 + i * 512, 512) for i in range(4)]
                mt = [carve_f32(28672 + i * 512, 512) for i in range(4)]
                bd = carve_f32(30720, D)
                assert 30720 + D <= ARENA_W
                accB = [[Buf() for _ in range(4)] for _ in range(8)]
                actB = [Buf() for _ in range(8)]
                xsTB = [Buf(), Buf()]
                xslB = [Buf(), Buf()]
                YH = Rot([(ysh[i], Buf()) for i in range(4)])
                SU = Rot([(mt[i], Buf()) for i in range(4)])
                bdB = Buf()
                dma(SP, semM, [], [bdB], lambda: nc.sync.dma_start(out=bd[0:NE, :], in_=b_dn_d[l]))
                op(DVE, [constB], [bglB], lambda: nc.vector.tensor_scalar(
                    out=bgl[:, :, 0:8], in0=cvec[:, l, 33:33 + 512].rearrange("p (e j) -> p e j", j=16)[:, :, 0:8], scalar1=1.702, scalar2=None, op0=ALU.mult))
                op(DVE, [constB], [bglB], lambda: nc.vector.tensor_scalar(
                    out=bgl[:, :, 8:16], in0=cvec[:, l, 33:33 + 512].rearrange("p (e j) -> p e j", j=16)[:, :, 8:16], scalar1=1.0, scalar2=None, op0=ALU.add))
                alt = [0]

                def evac(out_ap, in_ap, reads, writes):
                    alt[0] += 1
                    if alt[0] % 2:
                        op(ACT, reads, writes, lambda: nc.scalar.copy(out=out_ap, in_=in_ap))
                    else:
                        op(DVE, reads, writes, lambda: nc.vector.tensor_copy(out=out_ap, in_=in_ap))

                def load_xsl(e):
                    xi = e % 2
                    dma(SP, semXL[xi], [xsB], [xslB[xi]], lambda e=e, xi=xi: nc.sync.dma_start(
                        out=xsl[xi], in_=xs_d[e * CAP:(e + 1) * CAP, :].rearrange("(s p) k -> p s k", p=128)))

                def transposes(e):
                    xi = e % 2
                    for c in range(8):
                        tp, tpB = PS.next()
                        tpb = tp[:].bitcast(BF16)

                        def ftr2(tpb=tpb, c=c, xi=xi):
                            ins = None
                            for sc in range(NSC):
                                ins = nc.tensor.transpose(tpb[:, sc * 128:(sc + 1) * 128], xsl[xi][:, sc, c * 128:(c + 1) * 128], nsl[:, 12, :])
                            return ins
                        op(PE, [xslB[xi], constB], [tpB], ftr2)
                        evac(xsT[xi][:, c, :], tpb[:, 0:CAP], [tpB], [xsTB[xi]])

                PSU = Rot([(psum[i], psB_all[i]) for i in range(4)])
                PST = Rot([(psum[i], psB_all[i]) for i in (4, 5)])
                PSD = Rot([(psum[i], psB_all[i]) for i in (6, 7)])
                load_xsl(0)
                transposes(0)
                for e in range(NE):
                    xi = e % 2
                    if e + 1 < NE:
                        load_xsl(e + 1)
                    for uu in range(2):
                        sG = wload([(0, D, 512, w_up_d[l, e][:, uu * 512:(uu + 1) * 512])])
                        sL = wload([(0, D, 512, w_up_d[l, e][:, D + uu * 512:D + (uu + 1) * 512])])
                        Wg = rview(sG, 0, 8, 512)
                        Wl = rview(sL, 0, 8, 512)
                        for half in range(2):
                            stage = []
                            for jj in range(2):
                                jq = half * 2 + jj
                                j = 4 * uu + jq
                                pg, pgB = PS.next()
                                mm_group(pg[:, 0:CAP], pgB, [(Wg[:, kc, jq * 128:(jq + 1) * 128], xsT[xi][:, kc, :]) for kc in range(8)], [ringB[sG], xsTB[xi]])
                                pl, plB = PS.next()
                                mm_group(pl[:, 0:CAP], plB, [(Wl[:, kc, jq * 128:(jq + 1) * 128], xsT[xi][:, kc, :]) for kc in range(8)], [ringB[sL], xsTB[xi]])
                                s_, sB_ = SU.next()
                                u_, uB_ = SU.next()
                                s_ = s_[:, 0:CAP]
                                u_ = u_[:, 0:CAP]
                                op(ACT, [pgB, bglB], [sB_], lambda pg=pg, s_=s_, j=j: nc.scalar.activation(
                                    out=s_, in_=pg[:, 0:CAP], func=AF.Silu, bias=bgl[:, e, j:j + 1], scale=1.702))
                                op(ACT, [plB, bglB], [uB_], lambda pl=pl, u_=u_, j=j: nc.scalar.activation(
                                    out=u_, in_=pl[:, 0:CAP], func=AF.Identity, bias=bgl[:, e, 8 + j:9 + j], scale=1.0))
                                stage.append((j, s_, sB_, u_, uB_))
                            for (j, s_, sB_, u_, uB_) in stage:
                                op(DVE, [uB_], [uB_], lambda u_=u_: nc.vector.tensor_scalar(out=u_, in0=u_, scalar1=-6.0, scalar2=8.0, op0=ALU.max, op1=ALU.min))
                            for (j, s_, sB_, u_, uB_) in stage:
                                op(DVE, [sB_, uB_], [actB[j]], lambda s_=s_, u_=u_, j=j: nc.vector.scalar_tensor_tensor(
                                    out=actT[:, j, :], in0=s_, scalar=CS_SILU, in1=u_, op0=ALU.min, op1=ALU.mult))
                    if e + 1 < NE:
                        transposes(e + 1)
                    for d in range(2):
                        sD = wload([(0, D, 512, w_dn_d[l, e][:, d * 512:(d + 1) * 512])])
                        Wd = rview(sD, 0, 8, 512)
                        for sc in range(NSC):
                            py, pyB = PS.next()
                            mm_group(py[:], pyB, [(actT[:, f, sc * 128:(sc + 1) * 128], Wd[:, f, :]) for f in range(8)], [ringB[sD]] + actB)
                            yh, yhB = YH.next()
                            evac(yh, py[:], [pyB], [yhB])
                            r0 = e * CAP + sc * 128
                            dma(SP, semYS, [yhB], [ysB], lambda yh=yh, r0=r0, d=d: nc.sync.dma_start(
                                out=ys_d[r0:r0 + 128, d * 512:(d + 1) * 512], in_=yh))
                barrier()
                del slots[NS:]
                for c in range(8):
                    for tt in range(4):
                        ps, psB = PS.next()
                        op(PE, [bdB, GTB], [psB], lambda ps=ps, c=c, tt=tt: nc.tensor.matmul(
                            ps[:], lhsT=bd[0:NE, c * 128:(c + 1) * 128], rhs=GT[0:NE, tt * 512:(tt + 1) * 512], start=True, stop=True))
                        op(ACT, [psB], [accB[c][tt]], lambda ps=ps, c=c, tt=tt: nc.scalar.copy(out=acc[:, c, tt * 512:(tt + 1) * 512], in_=ps[:]))
                yg = [carve_f32(16384 + i * 4096, 4096).rearrange("p (k n) -> p k n", n=1024) for i in range(2)]
                ygB = [[Buf() for _ in range(4)] for _ in range(2)]
                for sidx in range(16):
                    yi = sidx % 2
                    tt = sidx // 4
                    cb0 = sidx * 128
                    for k in range(4):
                        dma(POOL, semYG[yi], [ysB, d4Bs[sidx]], [ygB[yi][k]], lambda yi=yi, k=k, sidx=sidx: nc.gpsimd.indirect_dma_start(
                            out=yg[yi][:, k, :], out_offset=None, in_=ys_d[:, :],
                            in_offset=bass.IndirectOffsetOnAxis(ap=D4[:, sidx, k:k + 1], axis=0)))
                    y0 = yg[yi][:, 0, :]
                    op(DVE, [g4Bs[sidx]], [ygB[yi][0]], lambda y0=y0, sidx=sidx: nc.vector.tensor_scalar(
                        out=y0, in0=y0, scalar1=G4[:, sidx, 0:1], scalar2=None, op0=ALU.mult))
                    for k in range(1, 4):
                        op(DVE, [g4Bs[sidx], ygB[yi][k]], [ygB[yi][0]], lambda y0=y0, yi=yi, k=k, sidx=sidx: nc.vector.scalar_tensor_tensor(
                            out=y0, in0=yg[yi][:, k, :], scalar=G4[:, sidx, k:k + 1], in1=y0, op0=ALU.mult, op1=ALU.add))
                    for half in range(2):
                        tp, tpB = PS.next()

                        def ftr3(tp=tp, y0=y0, half=half):
                            ins = None
                            for q in range(4):
                                cq = half * 4 + q
                                ins = nc.tensor.transpose(tp[:, q * 128:(q + 1) * 128], y0[:, cq * 128:(cq + 1) * 128], ident[:])
                            return ins
                        op(PE, [ygB[yi][0], constB], [tpB], ftr3)
                        asl = acc[:, half * 4:half * 4 + 4, cb0:cb0 + 128]
                        op(DVE, [tpB], [accB[half * 4 + q][tt] for q in range(4)], lambda tp=tp, asl=asl: nc.vector.tensor_tensor(
                            out=asl, in0=tp[:].rearrange("p (q n) -> p q n", n=128), in1=asl, op=ALU.add))
            else:
                barrier()
                o = 0
                acc = carve_f32(o, 8 * T).rearrange("p (c n) -> p c n", n=T); o += 8 * T
                actT = carve_bf(o, 8 * T).rearrange("p (c n) -> p c n", n=T); o += 8 * T // 2
                assert o == GT_OFF
                gbuf = [carve_f32(o + i * T, T) for i in range(2)]; o += 2 * T
                mt = [carve_f32(o + i * 512, 512) for i in range(4)]; o += 4 * 512
                bd = carve_f32(o, D); o += D
                assert o <= ARENA_W, o
                accB = [[Buf() for _ in range(4)] for _ in range(8)]
                actB = [[Buf() for _ in range(4)] for _ in range(8)]
                gbB = [GTB, Buf()]
                SU = Rot([(mt[i], Buf()) for i in range(4)])
                bdB = Buf()
                dma(SP, semM, [], [bdB], lambda: nc.sync.dma_start(out=bd[0:NE, :], in_=b_dn_d[l]))
                op(DVE, [constB], [bglB], lambda: nc.vector.tensor_scalar(
                    out=bgl[:, :, 0:8], in0=cvec[:, l, 33:33 + 512].rearrange("p (e j) -> p e j", j=16)[:, :, 0:8], scalar1=1.702, scalar2=None, op0=ALU.mult))
                op(DVE, [constB], [bglB], lambda: nc.vector.tensor_scalar(
                    out=bgl[:, :, 8:16], in0=cvec[:, l, 33:33 + 512].rearrange("p (e j) -> p e j", j=16)[:, :, 8:16], scalar1=1.0, scalar2=None, op0=ALU.add))
                for c in range(8):
                    for tt in range(4):
                        ps, psB = PS.next()
                        op(PE, [bdB, GTB], [psB], lambda ps=ps, c=c, tt=tt: nc.tensor.matmul(
                            ps[:], lhsT=bd[0:NE, c * 128:(c + 1) * 128], rhs=GT[0:NE, tt * 512:(tt + 1) * 512], start=True, stop=True))
                        op(ACT, [psB], [accB[c][tt]], lambda ps=ps, c=c, tt=tt: nc.scalar.copy(out=acc[:, c, tt * 512:(tt + 1) * 512], in_=ps[:]))
                for e in range(NE):
                    gi = e % 2
                    dma(SP, semGB[gi], [gTB], [gbB[gi]], lambda e=e, gi=gi: nc.sync.dma_start(
                        out=gbuf[gi], in_=gT_d[l][e:e + 1, :].partition_broadcast(128)))
                    for u in range(4):
                        sU = wload([(0, D, 256, w_up_d[l, e][:, u * 256:(u + 1) * 256]),
                                    (8 * 256, D, 256, w_up_d[l, e][:, D + u * 256:D + (u + 1) * 256])])
                        Wg = rview(sU, 0, 8, 256)
                        Wl = rview(sU, 8 * 256, 8, 256)
                        for tt in range(4):
                            xs = xb[:, :, tt * 512:(tt + 1) * 512]
                            stage = []
                            for jj in range(2):
                                j = 2 * u + jj
                                pg, pgB = PS.next()
                                mm_group(pg[:], pgB, [(Wg[:, kc, jj * 128:(jj + 1) * 128], xs[:, kc, :]) for kc in range(8)], [ringB[sU], xbB[tt]])
                                pl, plB = PS.next()
                                mm_group(pl[:], plB, [(Wl[:, kc, jj * 128:(jj + 1) * 128], xs[:, kc, :]) for kc in range(8)], [ringB[sU], xbB[tt]])
                                s_, sB_ = SU.next()
                                u_, uB_ = SU.next()
                                op(ACT, [pgB, bglB], [sB_], lambda pg=pg, s_=s_, j=j: nc.scalar.activation(
                                    out=s_, in_=pg[:], func=AF.Silu, bias=bgl[:, e, j:j + 1], scale=1.702))
                                op(ACT, [plB, bglB], [uB_], lambda pl=pl, u_=u_, j=j: nc.scalar.activation(
                                    out=u_, in_=pl[:], func=AF.Identity, bias=bgl[:, e, 8 + j:9 + j], scale=1.0))
                                stage.append((j, s_, sB_, u_, uB_))
                            for (j, s_, sB_, u_, uB_) in stage:
                                op(DVE, [uB_], [uB_], lambda u_=u_: nc.vector.tensor_scalar(out=u_, in0=u_, scalar1=-6.0, scalar2=8.0, op0=ALU.max, op1=ALU.min))
                            for (j, s_, sB_, u_, uB_) in stage:
                                op(DVE, [sB_, uB_], [sB_], lambda s_=s_, u_=u_: nc.vector.scalar_tensor_tensor(
                                    out=s_, in0=s_, scalar=CS_SILU, in1=u_, op0=ALU.min, op1=ALU.mult))
                            for (j, s_, sB_, u_, uB_) in stage:
                                op(DVE, [sB_, gbB[gi]], [actB[j][tt]], lambda s_=s_, j=j, tt=tt: nc.vector.scalar_tensor_tensor(
                                    out=actT[:, j, tt * 512:(tt + 1) * 512], in0=s_, scalar=1.0 / 1.702, in1=gbuf[gi][:, tt * 512:(tt + 1) * 512],
                                    op0=ALU.mult, op1=ALU.mult))
                    for d in range(2):
                        sD = wload([(0, D, 512, w_dn_d[l, e][:, d * 512:(d + 1) * 512])])
                        Wd = rview(sD, 0, 8, 512)
                        for tt in range(4):
                            for cc in range(4):
                                c = 4 * d + cc
                                py, pyB = PS.next()
                                mm_group(py[:], pyB, [(Wd[:, f, cc * 128:(cc + 1) * 128], actT[:, f, tt * 512:(tt + 1) * 512]) for f in range(8)],
                                         [ringB[sD]] + [actB[f][tt] for f in range(8)])
                                op(DVE, [pyB], [accB[c][tt]], lambda py=py, c=c, tt=tt: nc.vector.tensor_tensor(
                                    out=acc[:, c, tt * 512:(tt + 1) * 512], in0=py[:], in1=acc[:, c, tt * 512:(tt + 1) * 512], op=ALU.add))

            barrier()
            o = 8 * T
            ytile = carve_f32(o, 8 * 512).rearrange("p (c n) -> p c n", n=512); o += 8 * 512
            xres2 = [carve_f32(o + i * 4096, 8 * 512).rearrange("p (c n) -> p c n", n=512) for i in range(2)]; o += 2 * 8 * 512
            lt = [carve_f32(o + i * 512, 512) for i in range(5)]; o += 5 * 512
            assert o <= ARENA_W, o
            yB = [Buf() for _ in range(8)]
            xres2B = [Buf(), Buf()]
            semX2 = [semX, semXL[0]]
            lntmp = {"sq": Rot([(lt[0], Buf()), (lt[1], Buf())]), "mean": (lt[2], Buf()), "rstd": (lt[3], Buf()), "var": (lt[4], Buf())}
            dst = out_d if l == NL - 1 else xr_d[l][1]
            xr2B = Buf()
            st2 = {"lntmp": lntmp, "xrB": xr2B}
            def load_res(tt):
                dma(SP, semX2[tt % 2], [xr1B], [xres2B[tt % 2]], lambda tt=tt: nc.sync.dma_start(
                    out=xres2[tt % 2], in_=xr_d[l][0].rearrange("(c p) n -> p c n", p=128)[:, :, tt * 512:(tt + 1) * 512]))
            load_res(0)
            load_res(1)
            for tt in range(4):
                xres = xres2[tt % 2]
                xresB = xres2B[tt % 2]
                for c in range(8):
                    op(DVE, [accB[c][tt], xresB], [yB[c]], lambda c=c, tt=tt, xres=xres: nc.vector.scalar_tensor_tensor(
                        out=ytile[:, c, :], in0=xres[:, c, :], scalar=ALPHA, in1=acc[:, c, tt * 512:(tt + 1) * 512], op0=ALU.mult, op1=ALU.add))
                if tt + 2 < 4:
                    load_res(tt + 2)
                layer_norm_tile(l, 1, tt, ytile, yB, None, dst, st2)
            prev_xrB = xr2B
            barrier()
            if stop_after == f"moe{l}":
                dma(SP, semS, [], [], lambda: nc.sync.dma_start(out=dbg_d[:, 0:8 * T], in_=xb[:].rearrange("p a n -> p (a n)")))
                SP.wait(semS, semS.n)
                return nc

        dma(SP, semS, [flagB], [], lambda: nc.sync.dma_start(out=flag_d[:, :], in_=flagt[:]))
        SP.wait(semS, semS.n)
    return nc


def _alibi_slopes():
    n = 12
    return (2.0 ** (-8.0 * np.arange(1, n + 1) / n)).astype(np.float32)


def _host_consts(na_rpb):
    i = np.arange(128)[:, None]
    c = np.arange(WA)[None, :]
    r = i - c + 512
    sa = np.where(np.abs(r) <= 128, np.abs(r), 1.0e6).astype(np.float32)
    c = np.arange(WB)[None, :]
    sbv = np.abs(i - c + 1920).astype(np.float32)
    sl = _alibi_slopes()
    nsl = np.zeros((128, 13, 128), np.float32)
    eye = np.eye(128, dtype=np.float32)
    for h in range(12):
        nsl[:, h, :] = -sl[h] * eye
    nsl[:, 12, :] = eye
    rows = 32
    rr = np.arange(rows)
    rs = np.clip(rr - 4, 0, rows - 8)
    mr = np.where((rr[:, None] >= rs[None, :]) & (rr[:, None] < rs[None, :] + 8), 0.0, NEGBIG).astype(np.float32)
    tok_row = np.arange(T) // 64
    rowoh = (np.arange(rows)[:, None] == tok_row[None, :]).astype(np.float32)
    mrexp = mr[:, tok_row].astype(np.float32)
    cc = np.arange(64)
    cs = np.clip(cc - 8, 0, 48)
    colvalid = (cc[:, None] >= cs[None, :]) & (cc[:, None] < cs[None, :] + 16)
    dci = np.clip(cc[:, None] - cc[None, :] + 15, 0, 30)
    a = np.arange(2)[:, None]
    j = np.arange(22)[None, :]
    dr = a - j + 17
    drv = (dr >= 0) & (dr <= 14)
    drc = np.clip(dr, 0, 14)
    nl = na_rpb.shape[0]
    g = na_rpb[:, :, drc][:, :, :, :, dci]
    g = np.where(drv[None, None, :, :, None, None], g, np.float32(0.0))
    g = np.where(colvalid[None, None, None, None, :, :], g, np.float32(NEGBIG))
    g = np.ascontiguousarray(g.transpose(0, 1, 2, 4, 3, 5)).reshape(nl, 8, 128, 22 * 64).astype(np.float32)
    tris = (np.arange(128)[:, None] < np.arange(128)[None, :]).astype(np.float32)
    return dict(stripA=sa, stripB=sbv, nsl=nsl.reshape(128, 13 * 128), ident=eye, tris=tris, rowoh=rowoh, mrexp=mrexp, stripC=g)


def _prep_inputs(inp):
    f = lambda a: np.ascontiguousarray(np.asarray(a, dtype=np.float32))
    consts = _host_consts(f(inp["na_rpb"]))
    cvec = np.zeros((NL, 128, NCV), np.float32)
    brow = np.zeros((NL, 1, NBR), np.float32)
    for l in range(NL):
        cvec[l, :, 0:8] = f(inp["ln1_g"])[l].reshape(8, 128).T
        cvec[l, :, 8:16] = f(inp["ln1_b"])[l].reshape(8, 128).T
        cvec[l, :, 16:24] = f(inp["ln2_g"])[l].reshape(8, 128).T
        cvec[l, :, 24:32] = f(inp["ln2_b"])[l].reshape(8, 128).T
        cvec[l, :, 32] = f(inp["diff_norm_g"])[l]
        cvec[l, :, 33:] = f(inp["b_up"])[l].reshape(NE, 16, 128).transpose(2, 0, 1).reshape(128, NE * 16)
        brow[l, 0, 0:8] = f(inp["a_sink"])[l]
        brow[l, 0, 8:72] = f(inp["lambda_q1"])[l]
        brow[l, 0, 72:136] = f(inp["lambda_k1"])[l]
        brow[l, 0, 136:200] = f(inp["lambda_q2"])[l]
        brow[l, 0, 200:264] = f(inp["lambda_k2"])[l]
        brow[l, 0, 264:296] = f(inp["b_router"])[l]
        brow[l, 0, 296:328] = np.arange(NE, dtype=np.float32) * CAP + 1.0
    shared = dict(w_in=f(inp["w_in"]), w_branch=f(inp["w_branch"]), w_out=f(inp["w_out"]), w_router=f(inp["w_router"]),
                  w_up=f(inp["w_up"]), w_down=f(inp["w_down"]), b_down=f(inp["b_down"]), cvec=cvec, brow=brow, **consts)
    x = f(inp["x"])
    maps = []
    for b in range(8):
        m = dict(shared)
        m["xT"] = np.ascontiguousarray(x[b].T)
        maps.append(m)
    return maps


_NC_CACHE = {}


def kernel(**inputs):
    maps = _prep_inputs(inputs)
    if "sparse" not in _NC_CACHE:
        _NC_CACHE["sparse"] = build(sparse=True)
    res = run_bass_kernel_spmd(_NC_CACHE["sparse"], maps, core_ids=list(range(8)))
    _NC_CACHE["maxpos"] = max(float(np.max(np.asarray(res.results[b]["flag"]))) for b in range(8))
    if _NC_CACHE["maxpos"] > CAP - 0.5:
        if "dense" not in _NC_CACHE:
            _NC_CACHE["dense"] = build(sparse=False)
        res = run_bass_kernel_spmd(_NC_CACHE["dense"], maps, core_ids=list(range(8)))
    out = np.stack([np.ascontiguousarray(res.results[b]["outT"].T) for b in range(8)], axis=0)
    return out.astype(np.float32)
```

```python
import math
from contextlib import ExitStack
import numpy as np
import concourse.bass as bass
import concourse.mybir as mybir
from concourse.bass_utils import run_bass_kernel_spmd

F32 = mybir.dt.float32
BF16 = mybir.dt.bfloat16
ALU = mybir.AluOpType
AF = mybir.ActivationFunctionType
AX = mybir.AxisListType

T = 2048
D = 1024
NL = 2
NE = 32
ALPHA = (2 * NL) ** 0.25
LN_EPS = 1e-5
IN_W = 6912
NS = 4
NEGBIG = -30000.0
CS_SILU = 1.702 * 7.0 / (1.0 + math.exp(-1.702 * 7.0))
OFF_AQ, OFF_AK, OFF_AV = 0, 512, 640
OFF_BQ, OFF_BK, OFF_BV = 768, 1280, 1792
OFF_CQ, OFF_CK, OFF_CV = 2304, 2816, 3328
OFF_G = 3840
NCV = 33 + 512
NBR = 8 + 256 + 32 + 32
CAP = 384
NROW = NE * CAP + T
NSC = CAP // 128
I32 = mybir.dt.int32
WA, WB, WC = 1152, 3968, 22 * 64


class Sem:
    def __init__(self, h, name, total=False):
        self.h = h
        self.name = name
        self.n = 0
        self.total = total


class Buf:
    __slots__ = ("name", "w", "r")

    def __init__(self, name=""):
        self.name = name
        self.w = None
        self.r = {}


class Eng:
    def __init__(self, name, eng, sem, self_sync):
        self.name = name
        self.eng = eng
        self.sem = sem
        self.seen = {}
        self.self_sync = self_sync

    def wait(self, s, v):
        if s.total:
            v = max(v, s.n)
        if self.seen.get(s.name, 0) >= v:
            return
        self.eng.wait_ge(s.h, v)
        self.seen[s.name] = v


def _deps(E, reads, writes):
    deps = {}

    def need(tag):
        s, v = tag
        if s is E.sem and not E.self_sync:
            return
        if s.name not in deps or deps[s.name][1] < v:
            deps[s.name] = (s, v)

    for b in reads:
        if b.w is not None:
            need(b.w)
    for b in writes:
        if b.w is not None:
            need(b.w)
        for tag in b.r.values():
            need(tag)
    for s, v in deps.values():
        E.wait(s, v)


def op(E, reads, writes, fn):
    _deps(E, reads, writes)
    ins = fn()
    E.sem.n += 1
    ins.then_inc(E.sem.h, 1)
    tag = (E.sem, E.sem.n)
    for b in reads:
        b.r[E.sem.name] = tag
    for b in writes:
        b.w = tag
        b.r = {}


def dma(E, S, reads, writes, fn):
    _deps(E, reads, writes)
    ins = fn()
    S.n += 16
    ins.then_inc(S.h, 16)
    tag = (S, S.n)
    for b in reads:
        b.r[S.name] = tag
    for b in writes:
        b.w = tag
        b.r = {}


class Rot:
    def __init__(self, items):
        self.items = items
        self.i = 0

    def next(self):
        it = self.items[self.i % len(self.items)]
        self.i += 1
        return it


def build(stop_after=None, sparse=True):
    nc = bass.Bass("TRN2", target_bir_lowering=False)

    def din(name, shape):
        return nc.dram_tensor(name, list(shape), F32, kind="ExternalInput").ap()

    xT_d = din("xT", [D, T])
    w_in_d = din("w_in", [NL, D, IN_W])
    w_br_d = din("w_branch", [NL, 3, 512, D])
    w_out_d = din("w_out", [NL, D, D])
    w_rt_d = din("w_router", [NL, D, NE])
    NEW = 1 if (stop_after is not None and not stop_after.startswith("moe")) else NE
    w_up_d = din("w_up", [NL, NEW, D, 2 * D])
    w_dn_d = din("w_down", [NL, NEW, D, D])
    b_dn_d = din("b_down", [NL, NE, D])
    cvec_d = din("cvec", [NL, 128, NCV])
    brow_d = din("brow", [NL, 1, NBR])
    stripA_d = din("stripA", [128, WA])
    stripB_d = din("stripB", [128, WB])
    stripC_d = din("stripC", [NL, 8, 128, WC])
    nsl_d = din("nsl", [128, 13 * 128])
    ident_d = din("ident", [128, 128])
    tris_d = din("tris", [128, 128])
    rowoh_d = din("rowoh", [32, T])
    mrexp_d = din("mrexp", [32, T])
    out_d = nc.dram_tensor("outT", [D, T], F32, kind="ExternalOutput").ap()
    xr_d = [[nc.dram_tensor(f"xr{l}{k}", [D, T], F32, kind="Internal").ap() for k in range(2)] for l in range(NL)]
    gT_d = [nc.dram_tensor(f"gT{l}", [NE, T], F32, kind="Internal").ap() for l in range(NL)]
    flag_d = nc.dram_tensor("flag", [128, NL], F32, kind="ExternalOutput").ap()
    if sparse:
        xs_d = nc.dram_tensor("xs_scr", [NROW, D], BF16, kind="Internal").ap()
        ys_d = nc.dram_tensor("ys_scr", [NROW, D], F32, kind="Internal").ap()
    dbg_d = None
    if stop_after is not None:
        dbg_d = nc.dram_tensor("dbg", [128, 12 * T], BF16, kind="ExternalOutput").ap()
        dbg2_d = nc.dram_tensor("dbg2", [128, 8 * T], F32, kind="ExternalOutput").ap()

    with ExitStack() as es:
        EC = es.enter_context

        def sb(name, shape, dt):
            return EC(nc.sbuf_tensor("sb_" + name, list(shape), dt))

        def newsem(name, total=False):
            return Sem(EC(nc.semaphore(name)), name, total)

        PE = Eng("pe", nc.tensor, newsem("s_pe"), False)
        ACT = Eng("act", nc.scalar, newsem("s_act"), True)
        DVE = Eng("dve", nc.vector, newsem("s_dve"), True)
        POOL = Eng("pool", nc.gpsimd, newsem("s_pool"), True)
        SP = Eng("sp", nc.sync, newsem("s_sp"), True)
        ENGS = [PE, ACT, DVE, POOL, SP]
        semC = newsem("d_const", total=True)
        semCP = newsem("d_const_sw", total=True)
        semS = newsem("d_store", total=True)
        semG = newsem("d_gt", total=True)
        semX = newsem("d_xres")
        semGB = [newsem(f"d_gb{i}") for i in range(2)]
        semSC = [newsem(f"d_sc{i}") for i in range(2)]
        NSMAX = NS + 7
        semR = [newsem(f"d_ring{i}") for i in range(NSMAX)]
        semM = newsem("d_misc", total=True)
        semMP = newsem("d_misc_sw", total=True)
        semSCAT = newsem("d_scat", total=True)
        semYS = newsem("d_ys", total=True)
        semXL = [newsem(f"d_xl{i}") for i in range(2)]
        semYG = [newsem(f"d_yg{i}") for i in range(2)]
        DSEMS = [semC, semCP, semS, semG, semX, semM, semMP, semSCAT, semYS] + semGB + semSC + semR + semXL + semYG

        def barrier():
            for E in ENGS:
                for O in ENGS:
                    if O is not E and O.sem.n > 0:
                        E.wait(O.sem, O.sem.n)
                for S in DSEMS:
                    if S.n > 0:
                        E.wait(S, S.n)

        xb = sb("xb", [128, 8, T], BF16)
        xbB = [Buf(f"xb{t}") for t in range(4)]
        ring = sb("ring", [128, NS, 4096], BF16)
        ringB = [Buf(f"ring{i}") for i in range(NS + 7)]
        slots = [ring[:, i, :] for i in range(NS)]
        nsl = sb("nsl", [128, 13, 128], BF16)
        ident = sb("ident", [128, 128], F32)
        ones_bf = sb("ones_bf", [128, 128], BF16)
        ones_f = sb("ones_f", [128, 128], F32)
        cvec = sb("cvec", [128, NL, NCV], F32)
        brow = sb("brow", [128, NL, NBR], F32)
        smalls = sb("smalls", [128, 64], F32)
        bgl = sb("bgl", [128, NE, 16], F32)
        wr = sb("wr", [128, 8, NE], F32)
        tris = sb("tris", [128, 128], F32)
        cum = sb("cum", [128, NE], F32)
        ovmax = sb("ovmax", [128, NE], F32)
        D4 = sb("D4", [128, 16, 4], I32)
        G4 = sb("G4", [128, 16, 4], F32)
        flagt = sb("flagt", [128, NL], F32)
        cumB = Buf(); ovB = Buf(); flagB = Buf(); xsB = Buf(); ysB = Buf()
        d4Bs = [Buf() for _ in range(16)]
        g4Bs = [Buf() for _ in range(16)]
        constB = Buf("const")
        smallB = Buf("smalls")
        bglB = Buf("bgl")
        wrB = Buf("wr")
        ARENA_W = 31744
        arena = sb("arena", [128, ARENA_W], F32)

        def carve_f32(off, n):
            return arena[:, off:off + n]

        def carve_bf(off, n_bf):
            return arena[:, off:off + n_bf // 2].bitcast(BF16)

        psum = [EC(nc.psum_tensor(f"ps{i}", [128, 512], F32)) for i in range(8)]
        psB_all = [Buf(f"ps{i}") for i in range(8)]
        PS8 = Rot([(psum[i], psB_all[i]) for i in range(8)])
        PS4 = Rot([(psum[i], psB_all[i]) for i in range(4, 8)])
        PSACC = Rot([(psum[i], psB_all[i]) for i in range(4)])
        PSH = [PS8]

        class _PS:
            @staticmethod
            def next():
                return PSH[0].next()
        PS = _PS

        def cload(E, out_ap, in_ap):
            dma(E, (semCP if E is POOL else semC), [], [constB], lambda: E.eng.dma_start(out=out_ap, in_=in_ap))

        cload(POOL, nsl[:].rearrange("p a b -> p (a b)"), nsl_d[:, :])
        cload(SP, ident[:], ident_d[:, :])
        cload(SP, tris[:], tris_d[:, :])
        op(DVE, [], [flagB], lambda: nc.vector.memset(flagt[:], 0.0))
        for l in range(NL):
            cload(SP, cvec[:, l, :], cvec_d[l])
            cload(SP, brow[:, l, :], brow_d[l].partition_broadcast(128))
        op(DVE, [], [constB], lambda: nc.vector.memset(ones_f[:], 1.0))
        op(DVE, [], [constB], lambda: nc.vector.memset(ones_bf[:], 1.0))
        op(DVE, [], [smallB], lambda: nc.vector.memset(smalls[:, 0:1], LN_EPS))
        EPS = smalls[:, 0:1]
        for E_ in ENGS:
            E_.wait(semC, semC.n)
            E_.wait(semCP, semCP.n)

        ring_i = [0]

        def wload(pieces):
            s = ring_i[0] % len(slots)
            ring_i[0] += 1
            for (off, rows, ncols, src) in pieces:
                kc = rows // 128
                dst = slots[s][:, off:off + kc * ncols].rearrange("p (k n) -> p k n", n=ncols)
                dma(POOL, semR[s], [], [ringB[s]],
                    lambda dst=dst, src=src: nc.gpsimd.dma_start(out=dst, in_=src.rearrange("(k p) n -> p k n", p=128)))
            return s

        def rview(s, off, kc, ncols):
            return slots[s][:, off:off + kc * ncols].rearrange("p (k n) -> p k n", n=ncols)

        def mm_group(out_ap, psB, pairs, reads):
            def fn():
                ins = None
                n = len(pairs)
                for i, (l, r) in enumerate(pairs):
                    ins = nc.tensor.matmul(out_ap, lhsT=l, rhs=r, start=(i == 0), stop=(i == n - 1))
                return ins
            op(PE, reads, [psB], fn)

        def layer_norm_tile(l, which, tt, ytile, yB, xres_src_B, store_dst, st):
            gcol = 0 if which == 0 else 16
            tmp = st["lntmp"]
            s1, s1B = PS.next()
            s2, s2B = PS.next()
            mm_group(s1[:], s1B, [(ones_f[:], ytile[:, c, :]) for c in range(8)], yB + [constB])
            sqs = []
            for c in range(8):
                sq, sqB = tmp["sq"].next()
                op(ACT, [yB[c]], [sqB], lambda c=c, sq=sq: nc.scalar.activation(out=sq, in_=ytile[:, c, :], func=AF.Square))
                op(PE, [sqB, constB], [s2B],
                   lambda c=c, sq=sq: nc.tensor.matmul(s2[:], lhsT=ones_f[:], rhs=sq, start=(c == 0), stop=(c == 7)))
            mean, meanB = tmp["mean"]
            rstd, rstdB = tmp["rstd"]
            var, varB = tmp["var"]
            op(ACT, [s1B], [meanB], lambda: nc.scalar.activation(out=mean, in_=s1[:], func=AF.Identity, scale=1.0 / D))
            op(DVE, [meanB], [varB], lambda: nc.vector.tensor_tensor(out=var, in0=mean, in1=mean, op=ALU.mult))
            op(DVE, [s2B, varB], [varB],
               lambda: nc.vector.scalar_tensor_tensor(out=var, in0=s2[:], scalar=1.0 / D, in1=var, op0=ALU.mult, op1=ALU.subtract))
            op(ACT, [varB, smallB], [varB], lambda: nc.scalar.activation(out=var, in_=var, func=AF.Sqrt, bias=EPS, scale=1.0))
            op(DVE, [varB], [rstdB], lambda: nc.vector.reciprocal(out=rstd, in_=var))
            for c in range(8):
                yc = ytile[:, c, :]
                op(DVE, [meanB], [yB[c]], lambda yc=yc: nc.vector.tensor_tensor(out=yc, in0=yc, in1=mean, op=ALU.subtract))
            for c in range(8):
                yc = ytile[:, c, :]
                op(DVE, [rstdB], [yB[c]], lambda yc=yc: nc.vector.tensor_tensor(out=yc, in0=yc, in1=rstd, op=ALU.mult))
            for c in range(8):
                yc = ytile[:, c, :]
                op(ACT, [constB], [yB[c]],
                   lambda yc=yc, c=c: nc.scalar.activation(out=yc, in_=yc, func=AF.Identity,
                                                           bias=cvec[:, l, gcol + 8 + c:gcol + 9 + c],
                                                           scale=cvec[:, l, gcol + c:gcol + c + 1]))
            for c in range(8):
                yc = ytile[:, c, :]
                op(ACT, [yB[c]], [xbB[tt]],
                   lambda yc=yc, c=c: nc.scalar.copy(out=xb[:, c, tt * 512:(tt + 1) * 512], in_=yc))
            dma(SP, semS, yB, [st["xrB"]],
                lambda: nc.sync.dma_start(out=store_dst.rearrange("(c p) n -> p c n", p=128)[:, :, tt * 512:(tt + 1) * 512],
                                          in_=ytile))

        if sparse:
            zsrc = ring[:, NS - 1, :]
            op(DVE, [], [ringB[NS - 1]], lambda: nc.vector.memset(zsrc, 0.0))
            for i in range(NE):
                dma(SP, semM, [ringB[NS - 1]], [xsB], lambda i=i: nc.sync.dma_start(
                    out=xs_d[i * CAP:(i + 1) * CAP, :].rearrange("(s p) k -> p s k", p=128),
                    in_=zsrc.rearrange("p (s k) -> p s k", k=1024)[:, 0:NSC, :]))

        for tt in range(4):
            dma(POOL, semMP, [], [xbB[tt]],
                lambda tt=tt: nc.gpsimd.dma_start(out=xb[:, :, tt * 512:(tt + 1) * 512],
                                                  in_=xT_d.rearrange("(c p) n -> p c n", p=128)[:, :, tt * 512:(tt + 1) * 512]))

        prev_xrB = None
        for l in range(NL):
            lam_init = 0.8 - 0.6 * math.exp(-0.3 * l)
            res_src = xT_d if l == 0 else xr_d[l - 1][1]
            o = 0
            OT = carve_bf(o, 12 * T).rearrange("p (a n) -> p a n", n=T); o += 12 * T // 2
            stripA = carve_bf(o, WA); o += WA // 2
            stripB = carve_bf(o, WB); o += WB // 2
            R1 = o
            QT = [carve_bf(o + i * (T // 2), T) for i in range(2)]; o += T
            KT = [carve_bf(o + i * (T // 2), T) for i in range(2)]; o += T
            Vall = carve_bf(o, 16 * 512).rearrange("p (a n) -> p a n", n=512); o += 16 * 512 // 2
            Et = [carve_bf(o + i * 256, 512) for i in range(4)]; o += 4 * 256
            sC = [carve_bf(o + i * (WC // 2), WC) for i in range(2)]; o += WC
            ftmp = [carve_f32(o + i * 512, 512) for i in range(6)]; o += 6 * 512
            assert o <= ARENA_W, o
            otB = [[[Buf() for _ in range(4)] for _ in range(4)] for _ in range(3)]
            qB = [[Buf() for _ in range(4)] for _ in range(2)]
            kB = [Buf() for _ in range(2)]
            vB = [Buf() for _ in range(16)]
            ER = Rot([(Et[i], Buf()) for i in range(4)])
            sCB = [Buf(), Buf()]
            FT = Rot([(ftmp[i], Buf()) for i in range(6)])
            stripsB = Buf()

            dma(POOL, semMP, [], [stripsB], lambda: nc.gpsimd.dma_start(out=stripA, in_=stripA_d[:, :]))
            dma(POOL, semMP, [], [stripsB], lambda: nc.gpsimd.dma_start(out=stripB, in_=stripB_d[:, :]))

            cb = brow[:, l, :]
            sc1, sc1B = FT.next()
            op(DVE, [constB], [sc1B], lambda: nc.vector.tensor_tensor(out=sc1[:, 0:64], in0=cb[:, 8:72], in1=cb[:, 72:136], op=ALU.mult))
            op(DVE, [constB, sc1B], [sc1B], lambda: nc.vector.tensor_tensor(out=sc1[:, 64:128], in0=cb[:, 136:200], in1=cb[:, 200:264], op=ALU.mult))
            op(DVE, [sc1B], [sc1B], lambda: nc.vector.reduce_sum(out=sc1[:, 128:129], in_=sc1[:, 0:64], axis=AX.X))
            op(DVE, [sc1B], [sc1B], lambda: nc.vector.reduce_sum(out=sc1[:, 129:130], in_=sc1[:, 64:128], axis=AX.X))
            op(ACT, [sc1B], [sc1B], lambda: nc.scalar.activation(out=sc1[:, 130:132], in_=sc1[:, 128:130], func=AF.Exp))
            op(DVE, [sc1B], [sc1B], lambda: nc.vector.tensor_tensor(out=sc1[:, 132:133], in0=sc1[:, 131:132], in1=sc1[:, 130:131], op=ALU.subtract))
            op(DVE, [sc1B, smallB], [smallB], lambda: nc.vector.tensor_scalar(out=smalls[:, 1:2], in0=sc1[:, 132:133], scalar1=-lam_init, scalar2=None, op0=ALU.add))
            op(DVE, [constB, smallB], [smallB], lambda: nc.vector.tensor_scalar(out=smalls[:, 2:3], in0=cvec[:, l, 32:33], scalar1=(1.0 - lam_init), scalar2=None, op0=ALU.mult))
            op(ACT, [constB, smallB], [smallB], lambda: nc.scalar.activation(out=smalls[:, 8:16], in_=cb[:, 0:8], func=AF.Exp))
            NEGLAM = smalls[:, 1:2]
            GAINB = smalls[:, 2:3]

            win = w_in_d[l]
            PSH[0] = PS4

            def project_V(s, voff, ncols):
                for blk in range(16):
                    ps, psB = PS.next()
                    mm_group(ps[:, 0:ncols], psB,
                             [(xb[:, kc, blk * 128:(blk + 1) * 128], rview(s, 0, 8, 512)[:, kc, voff:voff + ncols]) for kc in range(8)],
                             [xbB[blk // 4], ringB[s]])
                    op(DVE, [psB], [vB[blk]], lambda ps=ps, blk=blk: nc.vector.tensor_copy(out=Vall[:, blk, 0:ncols], in_=ps[:, 0:ncols]))

            def project_T(s, coff, m, dst, dstB_list, scale, whole_buf=None):
                for tt in range(4):
                    ps, psB = PS.next()
                    mm_group(ps[0:m, :], psB,
                             [(rview(s, 0, 8, 512)[:, kc, coff:coff + m], xb[:, kc, tt * 512:(tt + 1) * 512]) for kc in range(8)],
                             [xbB[tt], ringB[s]])
                    wB = whole_buf if whole_buf is not None else dstB_list[tt]
                    if scale == 1.0:
                        op(DVE, [psB], [wB], lambda ps=ps, tt=tt: nc.vector.tensor_copy(out=dst[0:m, tt * 512:(tt + 1) * 512], in_=ps[0:m, :]))
                    else:
                        op(DVE, [psB], [wB], lambda ps=ps, tt=tt: nc.vector.tensor_scalar(out=dst[0:m, tt * 512:(tt + 1) * 512], in0=ps[0:m, :],
                                                                                         scalar1=scale, scalar2=None, op0=ALU.mult))

            def project_pair(s, coff, dsts, scale):
                for tt in range(4):
                    ps, psB = PS.next()
                    mm_group(ps[:, :], psB,
                             [(rview(s, 0, 8, 512)[:, kc, coff:coff + 128], xb[:, kc, tt * 512:(tt + 1) * 512]) for kc in range(8)],
                             [xbB[tt], ringB[s]])
                    for i, (dst, blist, whole) in enumerate(dsts):
                        wB = whole if whole is not None else blist[tt]
                        src = ps[i * 64:(i + 1) * 64, :]
                        if scale == 1.0:
                            op(DVE, [psB], [wB], lambda dst=dst, src=src, tt=tt: nc.vector.tensor_copy(out=dst[0:64, tt * 512:(tt + 1) * 512], in_=src))
                        else:
                            op(DVE, [psB], [wB], lambda dst=dst, src=src, tt=tt: nc.vector.tensor_scalar(
                                out=dst[0:64, tt * 512:(tt + 1) * 512], in0=src, scalar1=scale, scalar2=None, op0=ALU.mult))

            def attn_tile(kbs, kparts, qbuf, qBt, kbuf, kBk, bias_l, strip_ap_fn, stripBuf, v_fn, dv, t):
                num, numB = PSACC.next()
                den, denB = PSACC.next()
                nk = len(kbs)
                pend = []
                for i, kb in enumerate(kbs):
                    S, SB = PS.next()
                    lo, hi = kparts

                    def fs(S=S, kb=kb):
                        nc.tensor.matmul(S[:], lhsT=kbuf[lo:hi, kb * 128:(kb + 1) * 128], rhs=qbuf[lo:hi, t * 512:(t + 1) * 512],
                                         start=True, stop=False)
                        return nc.tensor.matmul(S[:], lhsT=bias_l, rhs=strip_ap_fn(kb), start=False, stop=True)
                    op(PE, [kBk, qBt, constB, stripBuf], [SB], fs)
                    E, EB = ER.next()
                    op(ACT, [SB], [EB], lambda S=S, E=E: nc.scalar.activation(out=E, in_=S[:], func=AF.Exp))
                    pend.append((i, kb, E, EB))
                    if len(pend) == 2 or i == nk - 1:
                        while pend and (len(pend) == 2 or i == nk - 1):
                            (pi, pkb, pE, pEB) = pend.pop(0)
                            op(PE, [pEB, vB[pkb]], [numB],
                               lambda pi=pi, pkb=pkb, pE=pE: nc.tensor.matmul(num[0:dv, :], lhsT=v_fn(pkb), rhs=pE, start=(pi == 0), stop=(pi == nk - 1)))
                            op(PE, [pEB, constB], [denB],
                               lambda pi=pi, pE=pE: nc.tensor.matmul(den[0:dv, :], lhsT=ones_bf[:, 0:dv], rhs=pE, start=(pi == 0), stop=(pi == nk - 1)))
                return num, numB, den, denB

            sA1 = wload([(0, D, 512, win[:, OFF_AQ:OFF_AQ + 512])])
            sA2 = wload([(0, D, 512, win[:, OFF_AK:OFF_AK + 512])])
            project_V(sA2, 128, 128)
            project_pair(sA2, 0, [(KT[0], None, kB[0]), (KT[1], None, kB[1])], 1.0)
            for g in range(2):
                for r in range(4):
                    h = g * 4 + r
                    qb = h % 2
                    if qb == 0:
                        project_pair(sA1, h * 64, [(QT[0], qB[0], None), (QT[1], qB[1], None)], 0.125)
                    for t in range(4):
                        kbs = [kb for kb in range(4 * t - 1, 4 * t + 5) if 0 <= kb < 16]
                        num, numB, den, denB = attn_tile(
                            kbs, (0, 64), QT[qb], qB[qb][t], KT[g % 2], kB[g % 2], nsl[:, h, :],
                            lambda kb, t=t: stripA[:, 512 - (128 * kb - 512 * t):512 - (128 * kb - 512 * t) + 512], stripsB,
                            lambda kb, g=g: Vall[:, kb, g * 64:(g + 1) * 64], 64, t)
                        rd, rdB = FT.next()
                        op(DVE, [denB, smallB], [rdB], lambda den=den, rd=rd, h=h: nc.vector.tensor_scalar(
                            out=rd[0:64, :], in0=den[0:64, :], scalar1=smalls[0:64, 8 + h:9 + h], scalar2=None, op0=ALU.add))
                        op(DVE, [rdB], [rdB], lambda rd=rd: nc.vector.reciprocal(out=rd[0:64, :], in_=rd[0:64, :]))
                        pb = (h % 2) * 64
                        op(DVE, [numB, rdB], [otB[0][h // 2][t]], lambda num=num, rd=rd, h=h, t=t, pb=pb: nc.vector.tensor_tensor(
                            out=OT[pb:pb + 64, h // 2, t * 512:(t + 1) * 512], in0=num[0:64, :], in1=rd[0:64, :], op=ALU.mult))

            sB1 = wload([(0, D, 512, win[:, OFF_BQ:OFF_BQ + 512])])
            sB2 = wload([(0, D, 512, win[:, OFF_BK:OFF_BK + 512])])
            sB3 = wload([(0, D, 512, win[:, OFF_BV:OFF_BV + 512])])
            project_V(sB3, 0, 512)
            for h in range(4):
                hb = h % 2
                project_T(sB2, h * 128, 128, KT[hb], None, 1.0, whole_buf=kB[hb])
                project_T(sB1, h * 128, 128, QT[hb], qB[hb], 0.125)
                for t in range(4):
                    oms = []
                    for m in range(2):
                        num, numB, den, denB = attn_tile(
                            list(range(16)), (m * 64, m * 64 + 64), QT[hb], qB[hb][t], KT[hb], kB[hb], nsl[:, 8 + h, :],
                            lambda kb, t=t: stripB[:, 1920 - (128 * kb - 512 * t):1920 - (128 * kb - 512 * t) + 512], stripsB,
                            lambda kb, h=h: Vall[:, kb, h * 128:(h + 1) * 128], 128, t)
                        om, omB = FT.next()
                        op(DVE, [denB], [omB], lambda den=den, om=om: nc.vector.reciprocal(out=om, in_=den[:]))
                        op(DVE, [numB, omB], [omB], lambda num=num, om=om: nc.vector.tensor_tensor(out=om, in0=num[:], in1=om, op=ALU.mult))
                        oms.append((om, omB))
                    (o0, o0B), (o1, o1B) = oms
                    op(DVE, [o1B, smallB], [o0B], lambda o0=o0, o1=o1: nc.vector.scalar_tensor_tensor(
                        out=o0, in0=o1, scalar=NEGLAM, in1=o0, op0=ALU.mult, op1=ALU.add))
                    op(ACT, [o0B], [o1B], lambda o0=o0, o1=o1: nc.scalar.activation(out=o1, in_=o0, func=AF.Square))
                    ss, ssB = PS.next()
                    op(PE, [o1B, constB], [ssB], lambda ss=ss, o1=o1: nc.tensor.matmul(ss[:], lhsT=ones_f[:], rhs=o1, start=True, stop=True))
                    op(ACT, [ssB, smallB], [o1B], lambda ss=ss, o1=o1: nc.scalar.activation(out=o1, in_=ss[:], func=AF.Sqrt, bias=EPS, scale=1.0 / 128))
                    op(DVE, [o1B], [o1B], lambda o1=o1: nc.vector.reciprocal(out=o1, in_=o1))
                    op(DVE, [o0B, o1B, smallB], [otB[1][h][t]], lambda o0=o0, o1=o1, h=h, t=t: nc.vector.scalar_tensor_tensor(
                        out=OT[:, 4 + h, t * 512:(t + 1) * 512], in0=o0, scalar=GAINB, in1=o1, op0=ALU.mult, op1=ALU.mult))

            for i in range(2):
                dma(POOL, semMP, [], [kB[i]], lambda i=i: nc.gpsimd.dma_start(out=KT[i][64:96, :], in_=rowoh_d[:, :]))
                for tt in range(4):
                    dma(POOL, semMP, [], [qB[i][tt]], lambda i=i, tt=tt: nc.gpsimd.dma_start(
                        out=QT[i][64:96, tt * 512:(tt + 1) * 512], in_=mrexp_d[:, tt * 512:(tt + 1) * 512]))
            sC1 = wload([(0, D, 512, win[:, OFF_CQ:OFF_CQ + 512])])
            sC2 = wload([(0, D, 512, win[:, OFF_CK:OFF_CK + 512])])
            sC3 = wload([(0, D, 512, win[:, OFF_CV:OFF_CV + 512])])
            project_V(sC3, 0, 512)
            for h in range(8):
                hb = h % 2
                dma(POOL, semSC[hb], [], [sCB[hb]], lambda h=h, hb=hb: nc.gpsimd.dma_start(out=sC[hb], in_=stripC_d[l, h]))
                if hb == 0:
                    project_pair(sC2, h * 64, [(KT[0], None, kB[0]), (KT[1], None, kB[1])], 1.0)
                    project_pair(sC1, h * 64, [(QT[0], qB[0], None), (QT[1], qB[1], None)], 0.125)
                for t in range(4):
                    kbs = [kb for kb in range(4 * t - 2, 4 * t + 6) if 0 <= kb < 16]
                    num, numB, den, denB = attn_tile(
                        kbs, (0, 96), QT[hb], qB[hb][t], KT[hb], kB[hb], nsl[:, 12, :],
                        lambda kb, t=t, hb=hb: sC[hb][:, (10 - (2 * kb - 8 * t)) * 64:(10 - (2 * kb - 8 * t)) * 64 + 512], sCB[hb],
                        lambda kb, h=h: Vall[:, kb, h * 64:(h + 1) * 64], 64, t)
                    rd, rdB = FT.next()
                    op(DVE, [denB], [rdB], lambda den=den, rd=rd: nc.vector.reciprocal(out=rd[0:64, :], in_=den[0:64, :]))
                    pb = (h % 2) * 64
                    op(DVE, [numB, rdB], [otB[2][h // 2][t]], lambda num=num, rd=rd, h=h, t=t, pb=pb: nc.vector.tensor_tensor(
                        out=OT[pb:pb + 64, 8 + h // 2, t * 512:(t + 1) * 512], in0=num[0:64, :], in1=rd[0:64, :], op=ALU.mult))

            if stop_after == f"attn{l}":
                barrier()
                dma(SP, semS, [], [], lambda: nc.sync.dma_start(out=dbg_d[:, :], in_=OT.rearrange("p a n -> p (a n)")))
                SP.wait(semS, semS.n)
                return nc

            barrier()
            PSH[0] = PS8
            mg = carve_bf(R1, 8 * T).rearrange("p (a n) -> p a n", n=T)
            o = R1 + 8 * T // 2
            mtmp = [carve_f32(o + i * 512, 512) for i in range(8)]; o += 8 * 512
            assert o <= ARENA_W
            MT = Rot([(mtmp[i], Buf()) for i in range(8)])
            mgB = [[Buf() for _ in range(4)] for _ in range(8)]
            for c in range(8):
                sG = wload([(i * 1024, D, 128, win[:, OFF_G + i * 1024 + c * 128:OFF_G + i * 1024 + (c + 1) * 128]) for i in range(3)])
                sW = wload([(i * 512, 512, 128, w_br_d[l, i][:, c * 128:(c + 1) * 128]) for i in range(3)])
                for tt in range(4):
                    prods = []
                    for i in range(3):
                        pg, pgB = PS.next()
                        mm_group(pg[:], pgB, [(rview(sG, i * 1024, 8, 128)[:, kc, :], xb[:, kc, tt * 512:(tt + 1) * 512]) for kc in range(8)],
                                 [ringB[sG], xbB[tt]])
                        pbp, pbB = PS.next()
                        mm_group(pbp[:], pbB, [(rview(sW, i * 512, 4, 128)[:, kc, :], OT[:, i * 4 + kc, tt * 512:(tt + 1) * 512]) for kc in range(4)],
                                 [ringB[sW]] + [otB[i][kc][tt] for kc in range(4)])
                        sg, sgB = MT.next()
                        op(ACT, [pgB], [sgB], lambda pg=pg, sg=sg: nc.scalar.activation(out=sg, in_=pg[:], func=AF.Sigmoid))
                        op(DVE, [pbB, sgB], [sgB], lambda pbp=pbp, sg=sg: nc.vector.tensor_tensor(out=sg, in0=pbp[:], in1=sg, op=ALU.mult))
                        prods.append((sg, sgB))
                    (p0, p0B), (p1, p1B), (p2, p2B) = prods
                    op(DVE, [p0B, p1B], [p0B], lambda p0=p0, p1=p1: nc.vector.tensor_tensor(out=p0, in0=p0, in1=p1, op=ALU.add))
                    op(DVE, [p0B, p2B], [mgB[c][tt]], lambda p0=p0, p2=p2, c=c, tt=tt: nc.vector.tensor_tensor(
                        out=mg[:, c, tt * 512:(tt + 1) * 512], in0=p0, in1=p2, op=ALU.add))

            barrier()
            o = 0
            ytile = carve_f32(o, 8 * 512).rearrange("p (c n) -> p c n", n=512); o += 8 * 512
            xres = carve_f32(o, 8 * 512).rearrange("p (c n) -> p c n", n=512); o += 8 * 512
            lt = [carve_f32(o + i * 512, 512) for i in range(5)]; o += 5 * 512
            rts = [carve_f32(o + i * 512, 512) for i in range(4)]; o += 4 * 512
            rtBs = [Buf() for _ in range(4)]
            xtl = [carve_bf(o + i * 512, 1024) for i in range(4)]; o += 4 * 512
            assert o <= R1, o
            XTR = Rot([(xtl[i], Buf()) for i in range(4)])
            if sparse:
                op(DVE, [cumB], [cumB], lambda: nc.vector.memset(cum[:], 0.0))
                op(DVE, [ovB], [ovB], lambda: nc.vector.memset(ovmax[:], 0.0))
            yB = [Buf() for _ in range(8)]
            xresB = Buf()
            lntmp = {"sq": Rot([(lt[0], Buf()), (lt[1], Buf())]), "mean": (lt[2], Buf()), "rstd": (lt[3], Buf()), "var": (lt[4], Buf())}
            GT_OFF = 8 * T + 8 * T // 2
            assert GT_OFF >= R1 + 8 * T // 2
            GT = carve_f32(GT_OFF, T)
            GTB = Buf()
            sO = [wload([(0, D, 512, w_out_d[l][:, d * 512:(d + 1) * 512])]) for d in range(2)]
            dma(SP, semM, [], [wrB], lambda: nc.sync.dma_start(out=wr[:], in_=w_rt_d[l].rearrange("(c p) e -> p c e", p=128)))
            xr1B = Buf()
            st1 = {"lntmp": lntmp, "xrB": xr1B}
            for tt in range(4):
                dma(SP, semX, ([prev_xrB] if prev_xrB is not None else []), [xresB], lambda tt=tt: nc.sync.dma_start(
                    out=xres, in_=res_src.rearrange("(c p) n -> p c n", p=128)[:, :, tt * 512:(tt + 1) * 512]))
                for c in range(8):
                    ps, psB = PS.next()
                    mm_group(ps[:], psB, [(rview(sO[c // 4], 0, 8, 512)[:, kc, (c % 4) * 128:(c % 4 + 1) * 128], mg[:, kc, tt * 512:(tt + 1) * 512])
                                          for kc in range(8)], [ringB[sO[c // 4]]] + [mgB[kc][tt] for kc in range(8)])
                    op(DVE, [psB, xresB], [yB[c]], lambda ps=ps, c=c: nc.vector.scalar_tensor_tensor(
                        out=ytile[:, c, :], in0=xres[:, c, :], scalar=ALPHA, in1=ps[:], op0=ALU.mult, op1=ALU.add))
                layer_norm_tile(l, 0, tt, ytile, yB, None, xr_d[l][0], st1)
                def router_sub(sub, tt=tt):
                    rt = rts[sub]
                    rtB = rtBs[sub]
                    lg, lgB = PS.next()
                    mm_group(lg[:, 0:NE], lgB, [(ytile[:, c, sub * 128:(sub + 1) * 128], wr[:, c, :]) for c in range(8)], yB + [wrB])
                    LG = rt[:, 0:32]; T8 = rt[:, 32:40]; NM = rt[:, 40:41]; EX = rt[:, 64:96]; GX = rt[:, 96:128]; DN = rt[:, 41:42]
                    op(DVE, [lgB, constB], [rtB], lambda: nc.vector.tensor_tensor(out=LG, in0=lg[:, 0:NE], in1=brow[:, l, 264:296], op=ALU.add))
                    yield
                    op(DVE, [rtB], [rtB], lambda: nc.vector.max(out=T8, in_=LG))
                    yield
                    op(DVE, [rtB], [rtB], lambda: nc.vector.tensor_scalar(out=NM, in0=T8[:, 0:1], scalar1=-1.0, scalar2=None, op0=ALU.mult))
                    yield
                    op(ACT, [rtB], [rtB], lambda: nc.scalar.activation(out=EX, in_=LG, func=AF.Exp, bias=NM, scale=1.0))
                    col = tt * 512 + sub * 128
                    sidx = tt * 4 + sub
                    if sparse:
                        MK = rt[:, 128:160]; VAL = rt[:, 160:192]; V8 = rt[:, 192:200]; OH = rt[:, 200:232]; PM = rt[:, 232:264]; D4F = rt[:, 264:268]
                        op(DVE, [rtB], [rtB], lambda: nc.vector.tensor_scalar(out=MK, in0=LG, scalar1=T8[:, 3:4], scalar2=None, op0=ALU.is_ge))
                        pp, ppB = PS.next()

                        def fpos():
                            nc.tensor.matmul(pp[:, 0:NE], lhsT=ones_f[:], rhs=cum[:], start=True, stop=False)
                            return nc.tensor.matmul(pp[:, 0:NE], lhsT=tris[:], rhs=MK, start=False, stop=True)
                        op(PE, [rtB, cumB, constB], [ppB], fpos)
                        op(DVE, [rtB, cumB], [cumB], lambda: nc.vector.tensor_tensor(out=cum[:], in0=cum[:], in1=MK, op=ALU.add))
                    yield
                    op(DVE, [rtB], [rtB], lambda: nc.vector.scalar_tensor_tensor(out=GX, in0=LG, scalar=T8[:, 3:4], in1=EX, op0=ALU.is_ge, op1=ALU.mult))
                    yield
                    op(DVE, [rtB], [rtB], lambda: nc.vector.reduce_sum(out=DN, in_=GX, axis=AX.X))
                    yield
                    op(DVE, [rtB], [rtB], lambda: nc.vector.reciprocal(out=DN, in_=DN))
                    yield
                    op(DVE, [rtB], [rtB], lambda: nc.vector.tensor_scalar(out=GX, in0=GX, scalar1=DN, scalar2=None, op0=ALU.mult))
                    yield
                    tp, tpB = PS.next()
                    op(PE, [rtB, constB], [tpB], lambda: nc.tensor.transpose(tp[0:NE, 0:128], GX, ident[:]))
                    yield
                    op(ACT, [tpB], [GTB], lambda: nc.scalar.copy(out=GT[0:NE, col:col + 128], in_=tp[0:NE, 0:128]))
                    if sparse:
                        op(DVE, [ppB, rtB], [rtB], lambda: nc.vector.tensor_tensor(out=PM, in0=pp[:, 0:NE], in1=MK, op=ALU.mult))
                        yield
                        op(DVE, [rtB, ovB], [ovB], lambda: nc.vector.tensor_tensor(out=ovmax[:], in0=ovmax[:], in1=PM, op=ALU.max))
                        op(DVE, [ppB, constB, rtB], [rtB], lambda: nc.vector.tensor_tensor(out=VAL, in0=pp[:, 0:NE], in1=brow[:, l, 296:328], op=ALU.add))
                        yield
                        op(DVE, [rtB], [rtB], lambda: nc.vector.tensor_tensor(out=VAL, in0=VAL, in1=MK, op=ALU.mult))
                        yield
                        op(DVE, [rtB], [rtB], lambda: nc.vector.max(out=V8, in_=VAL))
                        yield
                        op(DVE, [rtB], [rtB], lambda: nc.vector.tensor_scalar(out=D4F, in0=V8[:, 0:4], scalar1=-1.0, scalar2=None, op0=ALU.add))
                        yield
                        op(DVE, [rtB], [d4Bs[sidx]], lambda: nc.vector.tensor_copy(out=D4[:, sidx, :], in_=D4F))
                        tq, tqB = PS.next()
                        tqb = tq[:].bitcast(BF16)

                        def ftr():
                            ins = None
                            for c in range(8):
                                ins = nc.tensor.transpose(tqb[:, c * 128:(c + 1) * 128], xb[:, c, col:col + 128], nsl[:, 12, :])
                            return ins
                        op(PE, [xbB[tt], constB], [tqB], ftr)
                        yield
                        xt, xtB = XTR.next()
                        op(ACT, [tqB], [xtB], lambda: nc.scalar.copy(out=xt, in_=tqb))
                        for k in range(4):
                            op(DVE, [rtB], [rtB], lambda k=k: nc.vector.tensor_scalar(out=OH, in0=VAL, scalar1=V8[:, k:k + 1], scalar2=None, op0=ALU.is_equal))
                            yield
                            op(DVE, [rtB], [rtB], lambda: nc.vector.tensor_tensor(out=OH, in0=OH, in1=GX, op=ALU.mult))
                            yield
                            op(DVE, [rtB], [g4Bs[sidx]], lambda k=k: nc.vector.reduce_sum(out=G4[:, sidx, k:k + 1], in_=OH, axis=AX.X))
                            yield
                        for k in range(4):
                            dma(POOL, semSCAT, [xtB, d4Bs[sidx]], [xsB], lambda k=k: nc.gpsimd.indirect_dma_start(
                                out=xs_d[:, :], out_offset=bass.IndirectOffsetOnAxis(ap=D4[:, sidx, k:k + 1], axis=0), in_=xt, in_offset=None))

                gens = [router_sub(sub) for sub in range(4)]
                while gens:
                    for g_ in list(gens):
                        try:
                            next(g_)
                        except StopIteration:
                            gens.remove(g_)
            gTB = Buf()
            dma(SP, semG, [GTB], [gTB], lambda: nc.sync.dma_start(out=gT_d[l][:, :], in_=GT[0:NE, :]))
            if sparse:
                op(DVE, g4Bs, g4Bs, lambda: nc.vector.tensor_scalar(out=G4[:], in0=G4[:], scalar1=1.0 / 1.702, scalar2=None, op0=ALU.mult))
                op(DVE, [ovB], [ovB], lambda: nc.vector.reduce_max(out=ovmax[:, 0:1], in_=ovmax[:], axis=AX.X))
                op(DVE, [ovB, flagB], [flagB], lambda: nc.vector.tensor_copy(out=flagt[:, l:l + 1], in_=ovmax[:, 0:1]))

            if stop_after == f"ln1{l}":
                barrier()
                dma(SP, semS, [], [], lambda: nc.sync.dma_start(out=dbg_d[:, 0:8 * T], in_=xb[:].rearrange("p a n -> p (a n)")))
                dma(SP, semS, [], [], lambda: nc.sync.dma_start(out=dbg2_d[0:NE, 0:T], in_=GT[0:NE, :]))
                SP.wait(semS, semS.n)
                return nc

            if sparse:
                barrier()
                acc = carve_f32(0, 8 * T).rearrange("p (c n) -> p c n", n=T)
                for i in range(7):
                    slots.append(carve_bf(i * 2048, 4096))
                xsT = [carve_bf(16384 + i * 2048, 8 * CAP).rearrange("p (c n) -> p c n", n=CAP) for i in range(2)]
                xsl = [carve_bf(20480 + i * 2048, NSC * 1024).rearrange("p (s k) -> p s k", k=1024) for i in range(2)]
                assert GT_OFF == 24576
                actT = carve_bf(14336, 8 * CAP).rearrange("p (c n) -> p c n", n=CAP)
                ysh = [carve_f32(26624 + i * 512, 512) for i in range(4)]
                mt = [carve_f32(28672 + i * 512, 512) for i in range(4)]
                bd = carve_f32(30720, D)
                assert 30720 + D <= ARENA_W
                accB = [[Buf() for _ in range(4)] for _ in range(8)]
                actB = [Buf() for _ in range(8)]
                xsTB = [Buf(), Buf()]
                xslB = [Buf(), Buf()]
                YH = Rot([(ysh[i], Buf()) for i in range(4)])
                SU = Rot([(mt[i], Buf()) for i in range(4)])
                bdB = Buf()
                dma(SP, semM, [], [bdB], lambda: nc.sync.dma_start(out=bd[0:NE, :], in_=b_dn_d[l]))
                op(DVE, [constB], [bglB], lambda: nc.vector.tensor_scalar(
                    out=bgl[:, :, 0:8], in0=cvec[:, l, 33:33 + 512].rearrange("p (e j) -> p e j", j=16)[:, :, 0:8], scalar1=1.702, scalar2=None, op0=ALU.mult))
                op(DVE, [constB], [bglB], lambda: nc.vector.tensor_scalar(
                    out=bgl[:, :, 8:16], in0=cvec[:, l, 33:33 + 512].rearrange("p (e j) -> p e j", j=16)[:, :, 8:16], scalar1=1.0, scalar2=None, op0=ALU.add))
                alt = [0]

                def evac(out_ap, in_ap, reads, writes):
                    alt[0] += 1
                    if alt[0] % 2:
                        op(ACT, reads, writes, lambda: nc.scalar.copy(out=out_ap, in_=in_ap))
                    else:
                        op(DVE, reads, writes, lambda: nc.vector.tensor_copy(out=out_ap, in_=in_ap))

                def load_xsl(e):
                    xi = e % 2
                    dma(SP, semXL[xi], [xsB], [xslB[xi]], lambda e=e, xi=xi: nc.sync.dma_start(
                        out=xsl[xi], in_=xs_d[e * CAP:(e + 1) * CAP, :].rearrange("(s p) k -> p s k", p=128)))

                def transposes(e):
                    xi = e % 2
                    for c in range(8):
                        tp, tpB = PS.next()
                        tpb = tp[:].bitcast(BF16)

                        def ftr2(tpb=tpb, c=c, xi=xi):
                            ins = None
                            for sc in range(NSC):
                                ins = nc.tensor.transpose(tpb[:, sc * 128:(sc + 1) * 128], xsl[xi][:, sc, c * 128:(c + 1) * 128], nsl[:, 12, :])
                            return ins
                        op(PE, [xslB[xi], constB], [tpB], ftr2)
                        evac(xsT[xi][:, c, :], tpb[:, 0:CAP], [tpB], [xsTB[xi]])

                PSU = Rot([(psum[i], psB_all[i]) for i in range(4)])
                PST = Rot([(psum[i], psB_all[i]) for i in (4, 5)])
                PSD = Rot([(psum[i], psB_all[i]) for i in (6, 7)])
                load_xsl(0)
                transposes(0)
                for e in range(NE):
                    xi = e % 2
                    if e + 1 < NE:
                        load_xsl(e + 1)
                    for uu in range(2):
                        sG = wload([(0, D, 512, w_up_d[l, e][:, uu * 512:(uu + 1) * 512])])
                        sL = wload([(0, D, 512, w_up_d[l, e][:, D + uu * 512:D + (uu + 1) * 512])])
                        Wg = rview(sG, 0, 8, 512)
                        Wl = rview(sL, 0, 8, 512)
                        for half in range(2):
                            stage = []
                            for jj in range(2):
                                jq = half * 2 + jj
                                j = 4 * uu + jq
                                pg, pgB = PS.next()
                                mm_group(pg[:, 0:CAP], pgB, [(Wg[:, kc, jq * 128:(jq + 1) * 128], xsT[xi][:, kc, :]) for kc in range(8)], [ringB[sG], xsTB[xi]])
                                pl, plB = PS.next()
                                mm_group(pl[:, 0:CAP], plB, [(Wl[:, kc, jq * 128:(jq + 1) * 128], xsT[xi][:, kc, :]) for kc in range(8)], [ringB[sL], xsTB[xi]])
                                s_, sB_ = SU.next()
                                u_, uB_ = SU.next()
                                s_ = s_[:, 0:CAP]
                                u_ = u_[:, 0:CAP]
                                op(ACT, [pgB, bglB], [sB_], lambda pg=pg, s_=s_, j=j: nc.scalar.activation(
                                    out=s_, in_=pg[:, 0:CAP], func=AF.Silu, bias=bgl[:, e, j:j + 1], scale=1.702))
                                op(ACT, [plB, bglB], [uB_], lambda pl=pl, u_=u_, j=j: nc.scalar.activation(
                                    out=u_, in_=pl[:, 0:CAP], func=AF.Identity, bias=bgl[:, e, 8 + j:9 + j], scale=1.0))
                                stage.append((j, s_, sB_, u_, uB_))
                            for (j, s_, sB_, u_, uB_) in stage:
                                op(DVE, [uB_], [uB_], lambda u_=u_: nc.vector.tensor_scalar(out=u_, in0=u_, scalar1=-6.0, scalar2=8.0, op0=ALU.max, op1=ALU.min))
                            for (j, s_, sB_, u_, uB_) in stage:
                                op(DVE, [sB_, uB_], [actB[j]], lambda s_=s_, u_=u_, j=j: nc.vector.scalar_tensor_tensor(
                                    out=actT[:, j, :], in0=s_, scalar=CS_SILU, in1=u_, op0=ALU.min, op1=ALU.mult))
                    if e + 1 < NE:
                        transposes(e + 1)
                    for d in range(2):
                        sD = wload([(0, D, 512, w_dn_d[l, e][:, d * 512:(d + 1) * 512])])
                        Wd = rview(sD, 0, 8, 512)
                        for sc in range(NSC):
                            py, pyB = PS.next()
                            mm_group(py[:], pyB, [(actT[:, f, sc * 128:(sc + 1) * 128], Wd[:, f, :]) for f in range(8)], [ringB[sD]] + actB)
                            yh, yhB = YH.next()
                            evac(yh, py[:], [pyB], [yhB])
                            r0 = e * CAP + sc * 128
                            dma(SP, semYS, [yhB], [ysB], lambda yh=yh, r0=r0, d=d: nc.sync.dma_start(
                                out=ys_d[r0:r0 + 128, d * 512:(d + 1) * 512], in_=yh))
                barrier()
                del slots[NS:]
                for c in range(8):
                    for tt in range(4):
                        ps, psB = PS.next()
                        op(PE, [bdB, GTB], [psB], lambda ps=ps, c=c, tt=tt: nc.tensor.matmul(
                            ps[:], lhsT=bd[0:NE, c * 128:(c + 1) * 128], rhs=GT[0:NE, tt * 512:(tt + 1) * 512], start=True, stop=True))
                        op(ACT, [psB], [accB[c][tt]], lambda ps=ps, c=c, tt=tt: nc.scalar.copy(out=acc[:, c, tt * 512:(tt + 1) * 512], in_=ps[:]))
                yg = [carve_f32(16384 + i * 4096, 4096).rearrange("p (k n) -> p k n", n=1024) for i in range(2)]
                ygB = [[Buf() for _ in range(4)] for _ in range(2)]
                for sidx in range(16):
                    yi = sidx % 2
                    tt = sidx // 4
                    cb0 = sidx * 128
                    for k in range(4):
                        dma(POOL, semYG[yi], [ysB, d4Bs[sidx]], [ygB[yi][k]], lambda yi=yi, k=k, sidx=sidx: nc.gpsimd.indirect_dma_start(
                            out=yg[yi][:, k, :], out_offset=None, in_=ys_d[:, :],
                            in_offset=bass.IndirectOffsetOnAxis(ap=D4[:, sidx, k:k + 1], axis=0)))
                    y0 = yg[yi][:, 0, :]
                    op(DVE, [g4Bs[sidx]], [ygB[yi][0]], lambda y0=y0, sidx=sidx: nc.vector.tensor_scalar(
                        out=y0, in0=y0, scalar1=G4[:, sidx, 0:1], scalar2=None, op0=ALU.mult))
                    for k in range(1, 4):
                        op(DVE, [g4Bs[sidx], ygB[yi][k]], [ygB[yi][0]], lambda y0=y0, yi=yi, k=k, sidx=sidx: nc.vector.scalar_tensor_tensor(
                            out=y0, in0=yg[yi][:, k, :], scalar=G4[:, sidx, k:k + 1], in1=y0, op0=ALU.mult, op1=ALU.add))
                    for half in range(2):
                        tp, tpB = PS.next()

                        def ftr3(tp=tp, y0=y0, half=half):
                            ins = None
                            for q in range(4):
                                cq = half * 4 + q
                                ins = nc.tensor.transpose(tp[:, q * 128:(q + 1) * 128], y0[:, cq * 128:(cq + 1) * 128], ident[:])
                            return ins
                        op(PE, [ygB[yi][0], constB], [tpB], ftr3)
                        asl = acc[:, half * 4:half * 4 + 4, cb0:cb0 + 128]
                        op(DVE, [tpB], [accB[half * 4 + q][tt] for q in range(4)], lambda tp=tp, asl=asl: nc.vector.tensor_tensor(
                            out=asl, in0=tp[:].rearrange("p (q n) -> p q n", n=128), in1=asl, op=ALU.add))
            else:
                barrier()
                o = 0
                acc = carve_f32(o, 8 * T).rearrange("p (c n) -> p c n", n=T); o += 8 * T
                actT = carve_bf(o, 8 * T).rearrange("p (c n) -> p c n", n=T); o += 8 * T // 2
                assert o == GT_OFF
                gbuf = [carve_f32(o + i * T, T) for i in range(2)]; o += 2 * T
                mt = [carve_f32(o + i * 512, 512) for i in range(4)]; o += 4 * 512
                bd = carve_f32(o, D); o += D
                assert o <= ARENA_W, o
                accB = [[Buf() for _ in range(4)] for _ in range(8)]
                actB = [[Buf() for _ in range(4)] for _ in range(8)]
                gbB = [GTB, Buf()]
                SU = Rot([(mt[i], Buf()) for i in range(4)])
                bdB = Buf()
                dma(SP, semM, [], [bdB], lambda: nc.sync.dma_start(out=bd[0:NE, :], in_=b_dn_d[l]))
                op(DVE, [constB], [bglB], lambda: nc.vector.tensor_scalar(
                    out=bgl[:, :, 0:8], in0=cvec[:, l, 33:33 + 512].rearrange("p (e j) -> p e j", j=16)[:, :, 0:8], scalar1=1.702, scalar2=None, op0=ALU.mult))
                op(DVE, [constB], [bglB], lambda: nc.vector.tensor_scalar(
                    out=bgl[:, :, 8:16], in0=cvec[:, l, 33:33 + 512].rearrange("p (e j) -> p e j", j=16)[:, :, 8:16], scalar1=1.0, scalar2=None, op0=ALU.add))
                for c in range(8):
                    for tt in range(4):
                        ps, psB = PS.next()
                        op(PE, [bdB, GTB], [psB], lambda ps=ps, c=c, tt=tt: nc.tensor.matmul(
                            ps[:], lhsT=bd[0:NE, c * 128:(c + 1) * 128], rhs=GT[0:NE, tt * 512:(tt + 1) * 512], start=True, stop=True))
                        op(ACT, [psB], [accB[c][tt]], lambda ps=ps, c=c, tt=tt: nc.scalar.copy(out=acc[:, c, tt * 512:(tt + 1) * 512], in_=ps[:]))
                for e in range(NE):
                    gi = e % 2
                    dma(SP, semGB[gi], [gTB], [gbB[gi]], lambda e=e, gi=gi: nc.sync.dma_start(
                        out=gbuf[gi], in_=gT_d[l][e:e + 1, :].partition_broadcast(128)))
                    for u in range(4):
                        sU = wload([(0, D, 256, w_up_d[l, e][:, u * 256:(u + 1) * 256]),
                                    (8 * 256, D, 256, w_up_d[l, e][:, D + u * 256:D + (u + 1) * 256])])
                        Wg = rview(sU, 0, 8, 256)
                        Wl = rview(sU, 8 * 256, 8, 256)
                        for tt in range(4):
                            xs = xb[:, :, tt * 512:(tt + 1) * 512]
                            stage = []
                            for jj in range(2):
                                j = 2 * u + jj
                                pg, pgB = PS.next()
                                mm_group(pg[:], pgB, [(Wg[:, kc, jj * 128:(jj + 1) * 128], xs[:, kc, :]) for kc in range(8)], [ringB[sU], xbB[tt]])
                                pl, plB = PS.next()
                                mm_group(pl[:], plB, [(Wl[:, kc, jj * 128:(jj + 1) * 128], xs[:, kc, :]) for kc in range(8)], [ringB[sU], xbB[tt]])
                                s_, sB_ = SU.next()
                                u_, uB_ = SU.next()
                                op(ACT, [pgB, bglB], [sB_], lambda pg=pg, s_=s_, j=j: nc.scalar.activation(
                                    out=s_, in_=pg[:], func=AF.Silu, bias=bgl[:, e, j:j + 1], scale=1.702))
                                op(ACT, [plB, bglB], [uB_], lambda pl=pl, u_=u_, j=j: nc.scalar.activation(
                                    out=u_, in_=pl[:], func=AF.Identity, bias=bgl[:, e, 8 + j:9 + j], scale=1.0))
                                stage.append((j, s_, sB_, u_, uB_))
                            for (j, s_, sB_, u_, uB_) in stage:
                                op(DVE, [uB_], [uB_], lambda u_=u_: nc.vector.tensor_scalar(out=u_, in0=u_, scalar1=-6.0, scalar2=8.0, op0=ALU.max, op1=ALU.min))
                            for (j, s_, sB_, u_, uB_) in stage:
                                op(DVE, [sB_, uB_], [sB_], lambda s_=s_, u_=u_: nc.vector.scalar_tensor_tensor(
                                    out=s_, in0=s_, scalar=CS_SILU, in1=u_, op0=ALU.min, op1=ALU.mult))
                            for (j, s_, sB_, u_, uB_) in stage:
                                op(DVE, [sB_, gbB[gi]], [actB[j][tt]], lambda s_=s_, j=j, tt=tt: nc.vector.scalar_tensor_tensor(
                                    out=actT[:, j, tt * 512:(tt + 1) * 512], in0=s_, scalar=1.0 / 1.702, in1=gbuf[gi][:, tt * 512:(tt + 1) * 512],
                                    op0=ALU.mult, op1=ALU.mult))
                    for d in range(2):
                        sD = wload([(0, D, 512, w_dn_d[l, e][:, d * 512:(d + 1) * 512])])
                        Wd = rview(sD, 0, 8, 512)
                        for tt in range(4):
                            for cc in range(4):
                                c = 4 * d + cc
                                py, pyB = PS.next()
                                mm_group(py[:], pyB, [(Wd[:, f, cc * 128:(cc + 1) * 128], actT[:, f, tt * 512:(tt + 1) * 512]) for f in range(8)],
                                         [ringB[sD]] + [actB[f][tt] for f in range(8)])
                                op(DVE, [pyB], [accB[c][tt]], lambda py=py, c=c, tt=tt: nc.vector.tensor_tensor(
                                    out=acc[:, c, tt * 512:(tt + 1) * 512], in0=py[:], in1=acc[:, c, tt * 512:(tt + 1) * 512], op=ALU.add))

            barrier()
            o = 8 * T
            ytile = carve_f32(o, 8 * 512).rearrange("p (c n) -> p c n", n=512); o += 8 * 512
            xres2 = [carve_f32(o + i * 4096, 8 * 512).rearrange("p (c n) -> p c n", n=512) for i in range(2)]; o += 2 * 8 * 512
            lt = [carve_f32(o + i * 512, 512) for i in range(5)]; o += 5 * 512
            assert o <= ARENA_W, o
            yB = [Buf() for _ in range(8)]
            xres2B = [Buf(), Buf()]
            semX2 = [semX, semXL[0]]
            lntmp = {"sq": Rot([(lt[0], Buf()), (lt[1], Buf())]), "mean": (lt[2], Buf()), "rstd": (lt[3], Buf()), "var": (lt[4], Buf())}
            dst = out_d if l == NL - 1 else xr_d[l][1]
            xr2B = Buf()
            st2 = {"lntmp": lntmp, "xrB": xr2B}
            def load_res(tt):
                dma(SP, semX2[tt % 2], [xr1B], [xres2B[tt % 2]], lambda tt=tt: nc.sync.dma_start(
                    out=xres2[tt % 2], in_=xr_d[l][0].rearrange("(c p) n -> p c n", p=128)[:, :, tt * 512:(tt + 1) * 512]))
            load_res(0)
            load_res(1)
            for tt in range(4):
                xres = xres2[tt % 2]
                xresB = xres2B[tt % 2]
                for c in range(8):
                    op(DVE, [accB[c][tt], xresB], [yB[c]], lambda c=c, tt=tt, xres=xres: nc.vector.scalar_tensor_tensor(
                        out=ytile[:, c, :], in0=xres[:, c, :], scalar=ALPHA, in1=acc[:, c, tt * 512:(tt + 1) * 512], op0=ALU.mult, op1=ALU.add))
                if tt + 2 < 4:
                    load_res(tt + 2)
                layer_norm_tile(l, 1, tt, ytile, yB, None, dst, st2)
            prev_xrB = xr2B
            barrier()
            if stop_after == f"moe{l}":
                dma(SP, semS, [], [], lambda: nc.sync.dma_start(out=dbg_d[:, 0:8 * T], in_=xb[:].rearrange("p a n -> p (a n)")))
                SP.wait(semS, semS.n)
                return nc

        dma(SP, semS, [flagB], [], lambda: nc.sync.dma_start(out=flag_d[:, :], in_=flagt[:]))
        SP.wait(semS, semS.n)
    return nc


def _alibi_slopes():
    n = 12
    return (2.0 ** (-8.0 * np.arange(1, n + 1) / n)).astype(np.float32)


def _host_consts(na_rpb):
    i = np.arange(128)[:, None]
    c = np.arange(WA)[None, :]
    r = i - c + 512
    sa = np.where(np.abs(r) <= 128, np.abs(r), 1.0e6).astype(np.float32)
    c = np.arange(WB)[None, :]
    sbv = np.abs(i - c + 1920).astype(np.float32)
    sl = _alibi_slopes()
    nsl = np.zeros((128, 13, 128), np.float32)
    eye = np.eye(128, dtype=np.float32)
    for h in range(12):
        nsl[:, h, :] = -sl[h] * eye
    nsl[:, 12, :] = eye
    rows = 32
    rr = np.arange(rows)
    rs = np.clip(rr - 4, 0, rows - 8)
    mr = np.where((rr[:, None] >= rs[None, :]) & (rr[:, None] < rs[None, :] + 8), 0.0, NEGBIG).astype(np.float32)
    tok_row = np.arange(T) // 64
    rowoh = (np.arange(rows)[:, None] == tok_row[None, :]).astype(np.float32)
    mrexp = mr[:, tok_row].astype(np.float32)
    cc = np.arange(64)
    cs = np.clip(cc - 8, 0, 48)
    colvalid = (cc[:, None] >= cs[None, :]) & (cc[:, None] < cs[None, :] + 16)
    dci = np.clip(cc[:, None] - cc[None, :] + 15, 0, 30)
    a = np.arange(2)[:, None]
    j = np.arange(22)[None, :]
    dr = a - j + 17
    drv = (dr >= 0) & (dr <= 14)
    drc = np.clip(dr, 0, 14)
    nl = na_rpb.shape[0]
    g = na_rpb[:, :, drc][:, :, :, :, dci]
    g = np.where(drv[None, None, :, :, None, None], g, np.float32(0.0))
    g = np.where(colvalid[None, None, None, None, :, :], g, np.float32(NEGBIG))
    g = np.ascontiguousarray(g.transpose(0, 1, 2, 4, 3, 5)).reshape(nl, 8, 128, 22 * 64).astype(np.float32)
    tris = (np.arange(128)[:, None] < np.arange(128)[None, :]).astype(np.float32)
    return dict(stripA=sa, stripB=sbv, nsl=nsl.reshape(128, 13 * 128), ident=eye, tris=tris, rowoh=rowoh, mrexp=mrexp, stripC=g)


def _prep_inputs(inp):
    f = lambda a: np.ascontiguousarray(np.asarray(a, dtype=np.float32))
    consts = _host_consts(f(inp["na_rpb"]))
    cvec = np.zeros((NL, 128, NCV), np.float32)
    brow = np.zeros((NL, 1, NBR), np.float32)
    for l in range(NL):
        cvec[l, :, 0:8] = f(inp["ln1_g"])[l].reshape(8, 128).T
        cvec[l, :, 8:16] = f(inp["ln1_b"])[l].reshape(8, 128).T
        cvec[l, :, 16:24] = f(inp["ln2_g"])[l].reshape(8, 128).T
        cvec[l, :, 24:32] = f(inp["ln2_b"])[l].reshape(8, 128).T
        cvec[l, :, 32] = f(inp["diff_norm_g"])[l]
        cvec[l, :, 33:] = f(inp["b_up"])[l].reshape(NE, 16, 128).transpose(2, 0, 1).reshape(128, NE * 16)
        brow[l, 0, 0:8] = f(inp["a_sink"])[l]
        brow[l, 0, 8:72] = f(inp["lambda_q1"])[l]
        brow[l, 0, 72:136] = f(inp["lambda_k1"])[l]
        brow[l, 0, 136:200] = f(inp["lambda_q2"])[l]
        brow[l, 0, 200:264] = f(inp["lambda_k2"])[l]
        brow[l, 0, 264:296] = f(inp["b_router"])[l]
        brow[l, 0, 296:328] = np.arange(NE, dtype=np.float32) * CAP + 1.0
    shared = dict(w_in=f(inp["w_in"]), w_branch=f(inp["w_branch"]), w_out=f(inp["w_out"]), w_router=f(inp["w_router"]),
                  w_up=f(inp["w_up"]), w_down=f(inp["w_down"]), b_down=f(inp["b_down"]), cvec=cvec, brow=brow, **consts)
    x = f(inp["x"])
    maps = []
    for b in range(8):
        m = dict(shared)
        m["xT"] = np.ascontiguousarray(x[b].T)
        maps.append(m)
    return maps


_NC_CACHE = {}


def kernel(**inputs):
    maps = _prep_inputs(inputs)
    if "sparse" not in _NC_CACHE:
        _NC_CACHE["sparse"] = build(sparse=True)
    res = run_bass_kernel_spmd(_NC_CACHE["sparse"], maps, core_ids=list(range(8)))
    _NC_CACHE["maxpos"] = max(float(np.max(np.asarray(res.results[b]["flag"]))) for b in range(8))
    if _NC_CACHE["maxpos"] > CAP - 0.5:
        if "dense" not in _NC_CACHE:
            _NC_CACHE["dense"] = build(sparse=False)
        res = run_bass_kernel_spmd(_NC_CACHE["dense"], maps, core_ids=list(range(8)))
    out = np.stack([np.ascontiguousarray(res.results[b]["outT"].T) for b in range(8)], axis=0)
    return out.astype(np.float32)
```
